# Optimizing a Trainium2 kernel written in Bass

```python
import math
import jax, jax.numpy as jnp
from jax import lax
import numpy as np

D_MODEL = 2048
BATCH = 2
SEQ = 16384
DEPTH = 1

BLOCK = 128
EPS = 1e-6
SB_HEADS = 4
SB_HEAD_DIM = 128
SB_WIDTH = SB_HEADS * SB_HEAD_DIM
SW_HEADS = 16
SW_KV_HEADS = 2
SW_HEAD_DIM = 64
SW_WIDTH = SW_HEADS * SW_HEAD_DIM
SW_KV_WIDTH = SW_KV_HEADS * SW_HEAD_DIM
WINDOW = 128
OFF_Q_SB = 0
OFF_K_SB = OFF_Q_SB + SB_WIDTH
OFF_V_SB = OFF_K_SB + SB_WIDTH
OFF_Q_SW = OFF_V_SB + SB_WIDTH
OFF_K_SW = OFF_Q_SW + SW_WIDTH
OFF_V_SW = OFF_K_SW + SW_KV_WIDTH
OFF_G_SB = OFF_V_SW + SW_KV_WIDTH
OFF_G_SW = OFF_G_SB + D_MODEL
IN_WIDTH = OFF_G_SW + D_MODEL
N_GROUPS = 4
EXPERTS_PER_GROUP = 8
N_EXPERTS = N_GROUPS * EXPERTS_PER_GROUP
TOP_K_IN_GROUP = 2
D_EXPERT = 512
ROWS_PER_BLOCK = 512
NEG_INF = -1e30

kernel_name = "hybrid_stickbreak_swa_sink_hmoe"


def rms_norm(x, g):
    xf = x.astype(jnp.float32)
    y = xf * lax.rsqrt(jnp.mean(xf * xf, axis=-1, keepdims=True) + EPS)
    return (y * g.astype(jnp.float32)).astype(x.dtype)


def alibi_slopes(n_heads):
    h = jnp.arange(1, n_heads + 1, dtype=jnp.float32)
    return jnp.exp2(-8.0 * h / n_heads)


def stick_breaking_attention(q, k, v):
    b, s_len, h, d = q.shape
    nb = s_len // BLOCK
    scale = 1.0 / math.sqrt(d)
    qh = q.transpose(0, 2, 1, 3)
    kh = k.transpose(0, 2, 1, 3)
    vh = v.transpose(0, 2, 1, 3)
    outs = []
    for i in range(nb):
        n_keys = (i + 1) * BLOCK
        q_blk = qh[:, :, i * BLOCK:n_keys]
        k_pre = kh[:, :, :n_keys]
        v_pre = vh[:, :, :n_keys]
        z = jnp.einsum('bhqd,bhkd->bhqk', q_blk, k_pre).astype(jnp.float32) * scale
        q_pos = i * BLOCK + jnp.arange(BLOCK)
        mask = jnp.arange(n_keys)[None, :] < q_pos[:, None]
        log_beta = jax.nn.log_sigmoid(z)
        log_fail = jnp.where(mask, log_beta - z, 0.0)
        later = lax.cumsum(log_fail, axis=3, reverse=True) - log_fail
        a = jnp.where(mask, jnp.exp(log_beta + later), 0.0)
        outs.append(jnp.einsum('bhqk,bhkd->bhqd', a.astype(v_pre.dtype), v_pre))
    out = jnp.concatenate(outs, axis=2)
    return out.transpose(0, 2, 1, 3).reshape(b, s_len, h * d)


def sliding_window_attention(q, k, v, sinks):
    b, s_len, hq, d = q.shape
    kvh = k.shape[2]
    g = hq // kvh
    nb = s_len // BLOCK
    scale = 1.0 / math.sqrt(d)
    qb = q.reshape(b, nb, BLOCK, kvh, g, d)
    kb = k.reshape(b, nb, BLOCK, kvh, d)
    vb = v.reshape(b, nb, BLOCK, kvh, d)
    pad = ((0, 0), (1, 0), (0, 0), (0, 0), (0, 0))
    kc = jnp.concatenate([jnp.pad(kb[:, :-1], pad), kb], axis=2)
    vc = jnp.concatenate([jnp.pad(vb[:, :-1], pad), vb], axis=2)
    sc = jnp.einsum('bnqhgd,bnjhd->bnhgqj', qb, kc).astype(jnp.float32) * scale
    qi = jnp.arange(BLOCK)[:, None]
    kj = jnp.arange(2 * BLOCK)[None, :]
    dist = qi + BLOCK - kj
    key_abs = jnp.arange(nb)[:, None] * BLOCK - BLOCK + jnp.arange(2 * BLOCK)[None, :]
    mask = ((dist >= 0) & (dist < WINDOW))[None] & (key_abs >= 0)[:, None, :]
    slopes = alibi_slopes(hq).reshape(kvh, g)
    sc = sc - slopes[:, :, None, None] * dist.astype(jnp.float32)
    sc = jnp.where(mask[None, :, None, None], sc, NEG_INF)
    sink = sinks.astype(jnp.float32).reshape(kvh, g)[None, None, :, :, None, None]
    m = jnp.maximum(jnp.max(sc, axis=-1, keepdims=True), sink)
    p = jnp.exp(sc - m)
    p = p / (jnp.sum(p, axis=-1, keepdims=True) + jnp.exp(sink - m))
    o = jnp.einsum('bnhgqj,bnjhd->bnqhgd', p.astype(vc.dtype), vc)
    return o.reshape(b, s_len, hq * d)


def hierarchical_moe(h, w_router_group, b_router_group, w_router_expert, b_router_expert,
                     w_gate, w_up, w_down):
    b, s_len, d = h.shape
    t = b * s_len
    ht = h.reshape(t, d)
    group_prob = jax.nn.softmax((ht @ w_router_group).astype(jnp.float32) + b_router_group.astype(jnp.float32), axis=-1)
    p_grp, g_idx = lax.top_k(group_prob, 1)
    exp_logits = (ht @ w_router_expert).astype(jnp.float32) + b_router_expert.astype(jnp.float32)
    exp_logits = exp_logits.reshape(t, N_GROUPS, EXPERTS_PER_GROUP)
    sel_logits = jnp.take_along_axis(exp_logits, g_idx[:, :, None], axis=1)[:, 0]
    p_in = jax.nn.softmax(sel_logits, axis=-1)
    top_p, top_i = lax.top_k(p_in, TOP_K_IN_GROUP)
    top_p = top_p / jnp.sum(top_p, axis=-1, keepdims=True)
    expert_id = g_idx * EXPERTS_PER_GROUP + top_i
    gate = p_grp * top_p

    n_assign = t * TOP_K_IN_GROUP
    flat_e = expert_id.reshape(-1)
    flat_w = gate.reshape(-1)
    flat_tok = jnp.arange(n_assign, dtype=jnp.int32) // TOP_K_IN_GROUP
    order = jnp.argsort(flat_e, stable=True)
    e_sorted = flat_e[order]
    tok_sorted = flat_tok[order]
    w_sorted = flat_w[order]
    counts = jnp.bincount(flat_e, length=N_EXPERTS)
    padded = (counts + ROWS_PER_BLOCK - 1) // ROWS_PER_BLOCK * ROWS_PER_BLOCK
    start = jnp.cumsum(counts) - counts
    pend = jnp.cumsum(padded)
    pstart = pend - padded
    dest = pstart[e_sorted] + (jnp.arange(n_assign) - start[e_sorted])
    n_blocks = -(-n_assign // ROWS_PER_BLOCK) + N_EXPERTS
    n_rows = n_blocks * ROWS_PER_BLOCK
    row_tok = jnp.zeros((n_rows,), jnp.int32).at[dest].set(tok_sorted)
    row_w = jnp.zeros((n_rows,), jnp.float32).at[dest].set(w_sorted)
    block_e = jnp.clip(jnp.searchsorted(pend, jnp.arange(n_blocks) * ROWS_PER_BLOCK, side='right'),
                       0, N_EXPERTS - 1)
    x_rows = ht[row_tok].reshape(n_blocks, ROWS_PER_BLOCK, d)

    def expert_block(args):
        xb, e, wb = args
        a = xb @ w_gate[e]
        u = xb @ w_up[e]
        return ((jax.nn.silu(a) * u) @ w_down[e]) * wb[:, None].astype(xb.dtype)

    y_rows = lax.map(expert_block, (x_rows, block_e, row_w.reshape(n_blocks, ROWS_PER_BLOCK)))
    y = jax.ops.segment_sum(y_rows.reshape(n_rows, d), row_tok, num_segments=t)
    return y.reshape(b, s_len, d)


def setup_inputs(seed: int = 0) -> dict:
    key = jax.random.key(seed)
    ks = jax.random.split(key, 17)
    f32 = jnp.float32
    L, D = DEPTH, D_MODEL

    def nrm(k, shape, fan_in):
        return jax.random.normal(k, shape, f32) * (fan_in ** -0.5)

    return {
        "x": jax.random.normal(ks[0], (BATCH, SEQ, D), f32),
        "norm_mix": 1.0 + 0.02 * jax.random.normal(ks[1], (L, D), f32),
        "w_in": nrm(ks[2], (L, D, IN_WIDTH), D),
        "sinks": 0.5 * jax.random.normal(ks[3], (L, SW_HEADS), f32),
        "w_up_sb": nrm(ks[4], (L, SB_WIDTH, D), SB_WIDTH),
        "w_up_sw": nrm(ks[5], (L, SW_WIDTH, D), SW_WIDTH),
        "w_out": nrm(ks[6], (L, D, D), D),
        "norm_ffn": 1.0 + 0.02 * jax.random.normal(ks[7], (L, D), f32),
        "w_router_group": nrm(ks[8], (L, D, N_GROUPS), D),
        "b_router_group": 0.01 * jax.random.normal(ks[9], (L, N_GROUPS), f32),
        "w_router_expert": nrm(ks[10], (L, D, N_EXPERTS), D),
        "b_router_expert": 0.01 * jax.random.normal(ks[11], (L, N_EXPERTS), f32),
        "w_gate": nrm(ks[12], (L, N_EXPERTS, D, D_EXPERT), D),
        "w_up": nrm(ks[13], (L, N_EXPERTS, D, D_EXPERT), D),
        "w_down": nrm(ks[14], (L, N_EXPERTS, D_EXPERT, D), D_EXPERT),
        "norm_final": 1.0 + 0.02 * jax.random.normal(ks[15], (D,), f32),
    }


def reference(x, norm_mix, w_in, sinks, w_up_sb, w_up_sw, w_out, norm_ffn,
              w_router_group, b_router_group, w_router_expert, b_router_expert,
              w_gate, w_up, w_down, norm_final):
    b, s_len, _ = x.shape
    for layer in range(DEPTH):
        h = rms_norm(x, norm_mix[layer])
        proj = h @ w_in[layer]
        q_sb = proj[..., OFF_Q_SB:OFF_K_SB].reshape(b, s_len, SB_HEADS, SB_HEAD_DIM)
        k_sb = proj[..., OFF_K_SB:OFF_V_SB].reshape(b, s_len, SB_HEADS, SB_HEAD_DIM)
        v_sb = proj[..., OFF_V_SB:OFF_Q_SW].reshape(b, s_len, SB_HEADS, SB_HEAD_DIM)
        q_sw = proj[..., OFF_Q_SW:OFF_K_SW].reshape(b, s_len, SW_HEADS, SW_HEAD_DIM)
        k_sw = proj[..., OFF_K_SW:OFF_V_SW].reshape(b, s_len, SW_KV_HEADS, SW_HEAD_DIM)
        v_sw = proj[..., OFF_V_SW:OFF_G_SB].reshape(b, s_len, SW_KV_HEADS, SW_HEAD_DIM)
        gate_sb = jax.nn.sigmoid(proj[..., OFF_G_SB:OFF_G_SW])
        gate_sw = jax.nn.sigmoid(proj[..., OFF_G_SW:IN_WIDTH])
        y_sb = stick_breaking_attention(q_sb, k_sb, v_sb) @ w_up_sb[layer]
        y_sw = sliding_window_attention(q_sw, k_sw, v_sw, sinks[layer]) @ w_up_sw[layer]
        x = x + (gate_sb * y_sb + gate_sw * y_sw) @ w_out[layer]
        h2 = rms_norm(x, norm_ffn[layer])
        x = x + hierarchical_moe(h2, w_router_group[layer], b_router_group[layer],
                                 w_router_expert[layer], b_router_expert[layer],
                                 w_gate[layer], w_up[layer], w_down[layer])
    return rms_norm(x, norm_final)
```

```python
import contextlib
import numpy as np
import concourse.bass as bass
import concourse.mybir as mybir
from concourse.bass_utils import run_bass_kernel_spmd

F32 = mybir.dt.float32
BF16 = mybir.dt.bfloat16
I32 = mybir.dt.int32
AF = mybir.ActivationFunctionType
ALU = mybir.AluOpType
AX = mybir.AxisListType

ENGS = ("sp", "act", "pool", "dve", "pe")

D = 2048
S = 16384
NJ = 32
NG = 32
CAP = 384
NCT = CAP // 128
NE = 32
EPS = 1e-6
BIG = 1.0e4


class Tok:
    __slots__ = ("w", "rs")

    def __init__(self):
        self.w = None
        self.rs = {}


class Op:
    __slots__ = ("eng", "fn", "deps", "dma", "key", "tick", "semval", "has_dep", "idx")


class Prog:
    def __init__(self, nc):
        self.nc = nc
        self.ops = []
        self.key_last = {}
        self.key_cnt = {}
        self.last_on_eng = {}
        self.pending_barrier = {}
        self.kmap = {}

    def op(self, eng, fn, r=(), w=(), key=None):
        if key is not None:
            key = self.kmap.setdefault(key, len(self.kmap))
        o = Op()
        o.idx = len(self.ops)
        o.eng = eng
        o.fn = fn
        o.dma = key is not None
        o.key = key
        o.tick = None
        o.semval = None
        o.has_dep = False
        deps = set()
        for t in r:
            if t.w is not None:
                deps.add(t.w)
        for t in w:
            if t.w is not None:
                deps.add(t.w)
            deps.update(t.rs.values())
        if o.dma:
            if key in self.key_last:
                deps.add(self.key_last[key])
            self.key_last[key] = o.idx
            self.key_cnt[key] = self.key_cnt.get(key, 0) + 1
            o.semval = 16 * self.key_cnt[key]
        if eng in self.pending_barrier:
            deps.update(self.pending_barrier.pop(eng))
        deps.discard(o.idx)
        o.deps = deps
        rk = ("dma", o.idx) if o.dma else eng
        for t in r:
            t.rs[rk] = o.idx
        for t in w:
            t.w = o.idx
            t.rs = {}
        self.ops.append(o)
        if not o.dma:
            self.last_on_eng[eng] = o.idx
        return o

    def barrier(self):
        deps = set(self.last_on_eng.values()) | set(self.key_last.values())
        for e in ENGS:
            self.pending_barrier.setdefault(e, set()).update(deps)
        self.kmap = {}

    def emit(self, stack):
        nc = self.nc
        ops = self.ops
        for o in ops:
            for d in o.deps:
                p = ops[d]
                if p.dma or (p.eng == "pe" and o.eng == "pe"):
                    continue
                p.has_dep = True
        cnt = {e: 0 for e in ENGS}
        for o in ops:
            if not o.dma and o.has_dep:
                cnt[o.eng] += 1
                o.tick = cnt[o.eng]
        esem = {e: stack.enter_context(nc.semaphore("s_" + e)) for e in ENGS}
        ksem = {}
        for k in self.key_cnt:
            ksem[k] = stack.enter_context(nc.semaphore("k%d" % len(ksem)))
        by_eng = {e: [o for o in ops if o.eng == e] for e in ENGS}
        self.n_waits = 0

        def run(ename, e):
            seen = {}
            for o in by_eng[ename]:
                need = {}
                for d in o.deps:
                    p = ops[d]
                    if p.dma:
                        s, v = ksem[p.key], p.semval
                    else:
                        if p.eng == "pe" and ename == "pe":
                            continue
                        s, v = esem[p.eng], p.tick
                    if v > need.get(id(s), (None, 0))[1]:
                        need[id(s)] = (s, v)
                for s, v in need.values():
                    if seen.get(id(s), 0) < v:
                        e.wait_ge(s, v)
                        seen[id(s)] = v
                        self.n_waits += 1
                if o.fn is None:
                    continue
                ins = o.fn(e)
                if o.dma:
                    ins.then_inc(ksem[o.key], 16)
                elif o.has_dep:
                    ins.then_inc(esem[ename], 1)

        with nc.Block() as block:

            @block.sync
            def _(e):
                run("sp", e)

            @block.scalar
            def _(e):
                run("act", e)

            @block.gpsimd
            def _(e):
                run("pool", e)

            @block.vector
            def _(e):
                run("dve", e)

            @block.tensor
            def _(e):
                run("pe", e)


class Arena:
    def __init__(self, t, nelem):
        self.t = t
        self.n = nelem
        self.off = 0

    def alloc(self, n_elems, dt):
        nb = n_elems * (2 if dt == BF16 else 4)
        nb = (nb + 63) // 64 * 64
        a = self.off
        self.off += nb // 2
        assert self.off <= self.n, ("arena overflow", self.off, self.n)
        v = self.t[:, a:a + (n_elems * (2 if dt == BF16 else 4)) // 2]
        if dt != BF16:
            v = v.bitcast(dt)
        return v

    def mark(self):
        return self.off

    def release(self, m):
        self.off = m


def build_nc(dbg=False, stop_after=None):
    nc = bass.Bass("TRN2", target_bir_lowering=False)

    def din(name, shape, dt=F32):
        return nc.dram_tensor(name, list(shape), dt, kind="ExternalInput").ap()

    xb = din("xb", [128, 128, D])
    xo = din("xo", [NJ, 128, D])
    xp = din("xp", [NJ, 128, D])
    wA = din("wA", [D, 2816])
    wF = din("wF", [16, 128, 5632])
    wOut = din("wOut", [D, D])
    wR = din("wR", [D, 36])
    bR = din("bR", [1, 36])
    wGate = din("wGate", [NE, D, 512])
    wUp = din("wUp", [NE, D, 512])
    wDown = din("wDown", [NE, 512, D])
    nmix = din("nmix", [1, D])
    nffn = din("nffn", [1, D])
    nfin = din("nfin", [1, D])
    sinks = din("sinks", [1, 16])
    sbmask = din("sbmask", [128, 4, 128])
    swbias = din("swbias", [2, 128, 16, 256])
    erow = din("erow", [1, NE])
    out = nc.dram_tensor("out", [NJ, 128, D], F32, kind="ExternalOutput").ap()

    def scratch(name, shape, dt):
        return nc.dram_tensor(name, list(shape), dt, kind=("ExternalOutput" if (dbg and name in dbg) else "Internal")).ap()

    KTs = scratch("KTs", [NG, 128, 4 * 512], BF16)
    Vs = scratch("Vs", [NG, 128, 4 * 512], BF16)
    HT = scratch("HT", [NJ, 128, 16 * 128], BF16)
    QTsb = scratch("QTsb", [NJ, 128, 512], BF16)
    QTsw = scratch("QTsw", [NJ, 128, 1024], BF16)
    KTsw = scratch("KTsw", [NJ, 128, 256], BF16)
    Vsw = scratch("Vsw", [NJ, 128, 256], BF16)
    OTsb = scratch("OTsb", [NJ, 128, 512], BF16)
    OTsw = scratch("OTsw", [NJ, 128, 1024], BF16)
    X1 = scratch("X1", [NJ, 128, D], F32)
    XS = scratch("XS", [NE * CAP, D], BF16)
    YS = scratch("YS", [NE * CAP, D], F32)
    WFb = scratch("WFb", [16, 128, 5632], BF16)

    st = contextlib.ExitStack()
    with st:
        ARN = 102 * 1024
        arena_t = st.enter_context(nc.sbuf_tensor("arena", [128, ARN], BF16))
        AR = Arena(arena_t, ARN)
        PP = [st.enter_context(nc.psum_tensor("pp%d" % i, [128, 1024], F32)) for i in range(4)]
        TB = [Tok() for _ in range(8)]

        def bank(i):
            return PP[i // 2][:, (i % 2) * 512:(i % 2 + 1) * 512]

        P = Prog(nc)
        bc_cache = {}

        def bc_reg(e):
            if "r" not in bc_cache:
                rg = e.alloc_register("bcreg")
                e.reg_mov(rg, NE * CAP - 1)
                bc_cache["r"] = rg
            return bc_cache["r"]

        def DMA(eng, out_, in_, r, w, key, **kw):
            P.op(eng, lambda e: e.dma_start(out=out_, in_=in_, **kw), r=r, w=w, key=key)

        def MM(out_, lhsT, rhs, start, stop, r, w):
            P.op("pe", lambda e: e.matmul(out_, lhsT=lhsT, rhs=rhs, start=start, stop=stop), r=r, w=w)

        def ACT(out_, in_, func, r, w, **kw):
            P.op("act", lambda e: e.activation(out=out_, in_=in_, func=func, **kw), r=r, w=w)

        def TT(eng, out_, in0, in1, op, r, w):
            P.op(eng, lambda e: e.tensor_tensor(out=out_, in0=in0, in1=in1, op=op), r=r, w=w)

        def TS(eng, out_, in0, s1, s2, op0, op1, r, w, **kw):
            P.op(eng, lambda e: e.tensor_scalar(out=out_, in0=in0, scalar1=s1, scalar2=s2, op0=op0, op1=op1, **kw), r=r, w=w)

        def STT(out_, in0, scalar, in1, op0, op1, r, w, **kw):
            P.op("dve", lambda e: e.scalar_tensor_tensor(out=out_, in0=in0, scalar=scalar, in1=in1, op0=op0, op1=op1, **kw), r=r, w=w)

        identf = AR.alloc(128, F32)
        ident = AR.alloc(128, BF16)
        uincl = AR.alloc(128, BF16)
        ustrict = AR.alloc(128, BF16)
        ones = AR.alloc(128, BF16)
        mhalf = AR.alloc(2, F32)
        idx1 = AR.alloc(NJ, I32)
        idx2 = AR.alloc(NJ, I32)
        gt1 = AR.alloc(NJ, F32)
        gt2 = AR.alloc(NJ, F32)
        tC = Tok()

        def tri(dst, pattern, cm, cmp):
            P.op("pool", lambda e: e.memset(identf, 1.0), w=[tC])
            P.op("pool", lambda e: e.affine_select(out=identf, in_=identf, pattern=pattern, compare_op=cmp,
                                                   fill=0.0, base=0, channel_multiplier=cm), r=[tC], w=[tC])
            P.op("pool", lambda e: e.tensor_copy(out=dst, in_=identf), r=[tC], w=[tC])

        tri(ident, [[-1, 128]], 1, ALU.is_equal)
        tri(uincl, [[-1, 128]], 1, ALU.is_ge)
        tri(ustrict, [[1, 128]], -1, ALU.is_gt)
        P.op("pool", lambda e: e.memset(ones, 1.0), w=[tC])
        P.op("pool", lambda e: e.memset(mhalf[:, 0:1], -0.5), w=[tC])
        P.barrier()
        base_mark = AR.mark()

        class NormCtx:
            pass

        def make_norm_ctx(gvec_dram, keypfx, nslots=2, sep_junk=False):
            c = NormCtx()
            c.gbc = AR.alloc(D, F32)
            c.tg = Tok()
            DMA("sp", c.gbc, gvec_dram.to_broadcast([128, D]), [], [c.tg], keypfx + "g")
            c.nslots = nslots
            c.xt = [AR.alloc(D, F32) for _ in range(nslots)]
            c.txt = [Tok() for _ in range(nslots)]
            c.xn = [AR.alloc(D, BF16) for _ in range(2)]
            c.txn = [Tok() for _ in range(2)]
            c.st = [AR.alloc(2, F32) for _ in range(nslots)]
            c.tst = [Tok() for _ in range(nslots)]
            c.junk = AR.alloc(D, BF16) if sep_junk else None
            c.tjunk = Tok()
            c.n = 0
            c.nx = 0
            c.key = keypfx
            return c

        def norm_stats(c, s, xsrc, tx):
            if c.junk is not None:
                jk, tj = c.junk, c.tjunk
            else:
                jk, tj = c.xn[s % 2], c.txn[s % 2]
            STT(jk, xsrc, 1.0, xsrc, ALU.mult, ALU.mult, [tx], [tj, c.tst[s]], accum_out=c.st[s][:, 1:2])
            TS("pool", c.st[s][:, 1:2], c.st[s][:, 1:2], 1.0 / D, EPS, ALU.mult, ALU.add, [c.tst[s]], [c.tst[s]])
            TT("pool", c.st[s][:, 0:1], c.st[s][:, 1:2], mhalf[:, 0:1], ALU.pow, [c.tst[s]], [c.tst[s]])

        def transpose_tile(xn_ap, txn, pp_i, dst3, tdst, evac_eng):
            psT = PP[pp_i][:, :].bitcast(BF16)
            tb = [TB[2 * pp_i], TB[2 * pp_i + 1]]
            for cc in range(16):
                P.op("pe", lambda e, cc=cc: e.transpose(out=psT[:, cc * 128:(cc + 1) * 128],
                                                         in_=xn_ap[:, cc * 128:(cc + 1) * 128], identity=ident),
                     r=[txn], w=tb)
            src3 = psT.rearrange("p (c t) -> p c t", c=16)
            if evac_eng == "act":
                ACT(dst3, src3, AF.Copy, tb, [tdst])
            else:
                P.op("dve", lambda e: e.tensor_copy(out=dst3, in_=src3), r=tb, w=[tdst])

        def front1(c, src_dram):
            s = c.n % c.nslots
            c.n += 1
            DMA("sp", c.xt[s], src_dram, [], [c.txt[s]], c.key + "x%d" % s)
            norm_stats(c, s, c.xt[s], c.txt[s])
            return s

        def front2(c, s, dst3, tdst, pp_i, evac_eng="act"):
            xs = c.nx % 2
            c.nx += 1
            STT(c.xn[xs], c.xt[s], c.st[s][:, 0:1], c.gbc, ALU.mult, ALU.mult, [c.txt[s], c.tst[s], c.tg], [c.txn[xs]])
            transpose_tile(c.xn[xs], c.txn[xs], pp_i, dst3, tdst, evac_eng)

        class FrontPipe:
            def __init__(self, ctx, tiles):
                self.ctx = ctx
                self.tiles = tiles
                self.slot = {}

            def f1(self, k):
                if k < len(self.tiles) and k not in self.slot:
                    self.slot[k] = front1(self.ctx, self.tiles[k][0])

            def emit(self, k):
                self.f1(k)
                front2(self.ctx, self.slot[k], *self.tiles[k][1:])
                self.f1(k + 1)

        zt = AR.alloc(4096, BF16)
        tz = Tok()
        P.op("pool", lambda e: e.memset(zt, 0.0), w=[tz])
        XSz = XS.rearrange("(p r) d -> p (r d)", p=128)
        for zi in range(NE * CAP * D // 128 // 4096):
            DMA("pool", XSz[:, zi * 4096:(zi + 1) * 4096], zt, [tz], [], "Z%d" % (zi % 2))
        WA = AR.alloc(16 * 2816, BF16).rearrange("p (c n) -> p c n", c=16)
        tWA = Tok()
        wA3 = wA.rearrange("(c p) n -> p c n", p=128)
        for c0 in range(0, 2816, 704):
            DMA("pool", WA[:, :, c0:c0 + 704], wA3[:, :, c0:c0 + 704], [], [tWA], "WA")
        nA = make_norm_ctx(nmix, "A", nslots=3, sep_junk=True)
        hTg = [AR.alloc(16 * 512, BF16).rearrange("p (c t) -> p c t", c=16) for _ in range(2)]
        thT = [[Tok() for _ in range(4)] for _ in range(2)]
        KTst = [AR.alloc(4 * 512, BF16).rearrange("p (h t) -> p h t", h=4) for _ in range(2)]
        tKT = [[Tok() for _ in range(4)] for _ in range(2)]
        Vst = [AR.alloc(4 * 512, BF16).rearrange("p (t n) -> p t n", t=4) for _ in range(2)]
        tV = [[Tok() for _ in range(4)] for _ in range(2)]
        tKTs = [Tok() for _ in range(NG)]
        tVs = [Tok() for _ in range(NG)]
        pipeA = FrontPipe(nA, [(xb[k], hTg[(k // 4) % 2][:, :, (k % 4) * 128:(k % 4 + 1) * 128], thT[(k // 4) % 2][k % 4], k % 2)
                               for k in range(128)])
        for t in range(4):
            pipeA.emit(t)
        for g in range(NG):
            s = g % 2
            for t in range(4):
                if g + 1 < NG:
                    pipeA.emit(4 * (g + 1) + t)
                bk = 4 + (t % 2)
                for cc in range(16):
                    MM(bank(bk), WA[:, cc, t * 128:(t + 1) * 128], hTg[s][:, cc, :], cc == 0, cc == 15,
                       [tWA] + thT[s], [TB[bk]])
                ACT(KTst[s][:, t, :], bank(bk), AF.Copy, [TB[bk]], [tKT[s][t]])
                bv = 6 + (t % 2)
                for cc in range(16):
                    MM(bank(bv), hTg[s][:, cc, t * 128:(t + 1) * 128], WA[:, cc, 512:1024], cc == 0, cc == 15,
                       [tWA, thT[s][t]], [TB[bv]])
                ACT(Vst[s][:, t, :], bank(bv), AF.Copy, [TB[bv]], [tV[s][t]])
            DMA("pool", KTs[g], KTst[s].rearrange("p h t -> p (h t)"), tKT[s], [tKTs[g]], "KTst%d" % s)
            DMA("pool", Vs[g], Vst[s].rearrange("p t n -> p (t n)"), tV[s], [tVs[g]], "Vst%d" % s)

        QsbSt = [AR.alloc(512, BF16) for _ in range(2)]
        tQsbSt = [Tok() for _ in range(2)]
        QswSt = [AR.alloc(1024, BF16) for _ in range(2)]
        tQswSt = [Tok() for _ in range(2)]
        KswSt = [AR.alloc(256, BF16) for _ in range(2)]
        tKswSt = [Tok() for _ in range(2)]
        VswSt = [AR.alloc(256, BF16) for _ in range(2)]
        tVswSt = [Tok() for _ in range(2)]
        tHT = [Tok() for _ in range(NJ)]
        tQTsb = [Tok() for _ in range(NJ)]
        tQTsw = [Tok() for _ in range(NJ)]
        tKTsw = [Tok() for _ in range(NJ)]
        tVsw = [Tok() for _ in range(NJ)]

        tilesA2 = []
        for j in range(NJ):
            tilesA2.append((xp[j], hTg[j % 2][:, :, 0:128], thT[j % 2][0], 0))
            tilesA2.append((xo[j], hTg[j % 2][:, :, 128:256], thT[j % 2][1], 1))
        pipeA2 = FrontPipe(nA, tilesA2)

        def frontA2(j):
            pipeA2.emit(2 * j)
            pipeA2.emit(2 * j + 1)

        frontA2(0)
        for j in range(NJ):
            s = j % 2
            if j + 1 < NJ:
                frontA2(j + 1)
            DMA("pool", HT[j].rearrange("p (c t) -> p c t", c=16), hTg[s][:, :, 128:256], [thT[s][1]], [tHT[j]], "hTo%d" % s)
            for h in range(4):
                for cc in range(16):
                    MM(bank(4)[:, h * 128:(h + 1) * 128], WA[:, cc, 1024 + h * 128:1024 + (h + 1) * 128],
                       hTg[s][:, cc, 128:256], cc == 0, cc == 15, [tWA, thT[s][1]], [TB[4]])
            ACT(QsbSt[s], bank(4), AF.Copy, [TB[4]], [tQsbSt[s]])
            DMA("pool", QTsb[j], QsbSt[s], [tQsbSt[s]], [tQTsb[j]], "Qsb%d" % s)
            for gq in range(8):
                for cc in range(16):
                    MM(PP[3][:, gq * 128:(gq + 1) * 128], WA[:, cc, 1536 + gq * 128:1536 + (gq + 1) * 128],
                       hTg[s][:, cc, 128:256], cc == 0, cc == 15, [tWA, thT[s][1]], [TB[6], TB[7]])
            ACT(QswSt[s], PP[3][:, :], AF.Copy, [TB[6], TB[7]], [tQswSt[s]])
            DMA("pool", QTsw[j], QswSt[s], [tQswSt[s]], [tQTsw[j]], "Qsw%d" % s)
            for cc in range(16):
                MM(bank(5)[:, 0:256], WA[:, cc, 2560:2688], hTg[s][:, cc, 0:256], cc == 0, cc == 15,
                   [tWA, thT[s][0], thT[s][1]], [TB[5]])
            for wch in range(2):
                for cc in range(16):
                    MM(bank(5)[:, 256 + wch * 128:256 + (wch + 1) * 128], hTg[s][:, cc, wch * 128:(wch + 1) * 128],
                       WA[:, cc, 2688:2816], cc == 0, cc == 15, [tWA, thT[s][wch]], [TB[5]])
            ACT(KswSt[s], bank(5)[:, 0:256], AF.Copy, [TB[5]], [tKswSt[s]])
            ACT(VswSt[s], bank(5)[:, 256:512], AF.Copy, [TB[5]], [tVswSt[s]])
            DMA("pool", KTsw[j], KswSt[s], [tKswSt[s]], [tKTsw[j]], "Ksw%d" % s)
            DMA("pool", Vsw[j], VswSt[s], [tVswSt[s]], [tVsw[j]], "Vsw%d" % s)

        P.barrier()
        AR.release(base_mark)
        if stop_after == "A":
            return finish(nc, P, st, out, None)

        Msb = AR.alloc(512, F32).rearrange("p (t q) -> p t q", t=4)
        tM = Tok()
        DMA("sp", Msb, sbmask, [], [tM], "Cc")
        biasT = [AR.alloc(16 * 256, F32).rearrange("p (h k) -> p h k", h=16) for _ in range(2)]
        tBias = Tok()
        for v in range(2):
            DMA("sp", biasT[v], swbias[v], [], [tBias], "Cc")
        sinkb = AR.alloc(16, F32)
        tSink = Tok()
        DMA("sp", sinkb, sinks.to_broadcast([128, 16]), [], [tSink], "Cc")
        Qsb = [AR.alloc(512, BF16) for _ in range(2)]
        tQsb = [Tok() for _ in range(2)]
        NKV = 6
        KTg = [AR.alloc(2048, BF16).rearrange("p (h t) -> p h t", h=4) for _ in range(NKV)]
        tKTg = [Tok() for _ in range(NKV)]
        Vg = [AR.alloc(2048, BF16).rearrange("p (t n) -> p t n", t=4) for _ in range(NKV)]
        tVg = [Tok() for _ in range(NKV)]
        eb = [AR.alloc(1024, F32) for _ in range(4)]
        teb = [Tok() for _ in range(4)]
        spb = [AR.alloc(1024, BF16) for _ in range(4)]
        tspb = [Tok() for _ in range(4)]
        accb = [AR.alloc(512, BF16) for _ in range(4)]
        taccb = [Tok() for _ in range(4)]
        sumb = [AR.alloc(512, BF16) for _ in range(4)]
        tsumb = [Tok() for _ in range(4)]
        E2b = [AR.alloc(1024, F32) for _ in range(2)]
        tE2b = [Tok() for _ in range(2)]
        aTb = [AR.alloc(1024, BF16) for _ in range(4)]
        taTb = [Tok() for _ in range(4)]
        OsbSt = [AR.alloc(512, BF16) for _ in range(2)]
        tOsbSt = [Tok() for _ in range(2)]
        tOTsb = [Tok() for _ in range(NJ)]
        tOTsw = [Tok() for _ in range(NJ)]
        Qsw = [AR.alloc(1024, BF16).rearrange("p (g q) -> p g q", g=8) for _ in range(2)]
        tQsw = [Tok() for _ in range(2)]
        Kbd = [AR.alloc(512, BF16) for _ in range(2)]
        tKbd = [Tok() for _ in range(2)]
        Vp = [[AR.alloc(256, BF16).rearrange("p (w n) -> p w n", w=2) for _ in range(2)] for _ in range(2)]
        tVp = [Tok() for _ in range(2)]
        scb = [AR.alloc(512, F32) for _ in range(4)]
        tscb = [Tok() for _ in range(4)]
        pb = [AR.alloc(512, F32) for _ in range(4)]
        tpb = [Tok() for _ in range(4)]
        pnb = [AR.alloc(512, BF16) for _ in range(4)]
        tpnb = [Tok() for _ in range(4)]
        pTb = [AR.alloc(512, BF16) for _ in range(4)]
        tpTb = [Tok() for _ in range(4)]
        smal = [AR.alloc(16, F32) for _ in range(4)]
        tsmal = [Tok() for _ in range(4)]
        OswSt = [AR.alloc(1024, BF16) for _ in range(2)]
        tOswSt = [Tok() for _ in range(2)]
        for s in range(2):
            P.op("pool", lambda e, s=s: e.memset(Kbd[s], 0.0), w=[tKbd[s]])
            for kv in range(2):
                P.op("pool", lambda e, s=s, kv=kv: e.memset(Vp[s][kv].rearrange("p w n -> p (w n)"), 0.0), w=[tVp[s]])

        SCALE_SB = 1.0 / np.sqrt(128.0)
        SCALE_SW = 1.0 / 8.0
        nswa = [0]

        WFtmp = [AR.alloc(5632, BF16) for _ in range(2)]
        tWFtmp = [[Tok() for _ in range(3)] for _ in range(2)]

        def precast_step(j):
            if 1 <= j <= 16:
                f = j - 1
                DMA("pool", WFb[f], WFtmp[f % 2], tWFtmp[f % 2], [], "CWFo%d" % (f % 2))
            if j < 16:
                f = j
                for pi, (c0, c1) in enumerate(((0, 2048), (2048, 4096), (4096, 5632))):
                    DMA("pool", WFtmp[f % 2][:, c0:c1], wF[f][:, c0:c1], [], [tWFtmp[f % 2][pi]], "CWFi%d_%d" % (f % 2, pi))

        NSW = 4

        def swa_micro(j, g, G):
            s = j % 2
            bT = biasT[0 if j == 0 else 1]
            k = G % NSW
            half, gi = g // 4, g % 4
            sm = smal[k]
            sc3 = scb[k].rearrange("p (h k) -> p h k", h=2)
            tk = tsmal[k]

            def loads():
                precast_step(j)
                DMA("sp", Qsw[s].rearrange("p g q -> p (g q)"), QTsw[j], [tQTsw[j]], [tQsw[s]], "CQsw%d" % s)
                DMA("sp", Kbd[s][0:64, 0:256], KTsw[j][0:64, :], [tKTsw[j]], [tKbd[s]], "CKbd%d" % s)
                DMA("sp", Kbd[s][64:128, 256:512], KTsw[j][64:128, :], [tKTsw[j]], [tKbd[s]], "CKbd%d" % s)
                vsrc = Vsw[j].rearrange("p (w n) -> p w n", w=2)
                DMA("sp", Vp[s][0][:, :, 0:64], vsrc[:, :, 0:64], [tVsw[j]], [tVp[s]], "CVp%d" % s)
                DMA("sp", Vp[s][1][:, :, 64:128], vsrc[:, :, 64:128], [tVsw[j]], [tVp[s]], "CVp%d" % s)

            def m0():
                if g == 0:
                    loads()
                MM(bank(5), Qsw[s][:, g, :], Kbd[s], True, True, [tQsw[s], tKbd[s]], [TB[5]])

            def m1():
                STT(sc3, bank(5).rearrange("p (h k) -> p h k", h=2), SCALE_SW, bT[:, 2 * g:2 * g + 2, :],
                    ALU.mult, ALU.add, [TB[5], tBias], [tscb[k]])

            def m2():
                P.op("dve", lambda e: e.tensor_reduce(out=sm[:, 0:2], in_=sc3, axis=AX.X, op=ALU.max), r=[tscb[k]], w=[tk])

            def m3():
                TT("dve", sm[:, 0:2], sm[:, 0:2], sinkb[:, 2 * g:2 * g + 2], ALU.max, [tk, tSink], [tk])

            def m4():
                TS("dve", sm[:, 2:4], sm[:, 0:2], -1.0, None, ALU.mult, ALU.bypass, [tk], [tk])

            def m5():
                TT("dve", sm[:, 6:8], sinkb[:, 2 * g:2 * g + 2], sm[:, 2:4], ALU.add, [tk, tSink], [tk])

            def m6():
                for hh in range(2):
                    ACT(pb[k][:, hh * 256:(hh + 1) * 256], scb[k][:, hh * 256:(hh + 1) * 256], AF.Exp,
                        [tscb[k], tk], [tpb[k], tk], bias=sm[:, 2 + hh:3 + hh], accum_out=sm[:, 4 + hh:5 + hh])
                ACT(sm[:, 6:8], sm[:, 6:8], AF.Exp, [tk], [tk])

            def m7():
                TT("dve", sm[:, 8:10], sm[:, 6:8], sm[:, 4:6], ALU.add, [tk], [tk])

            def m8():
                P.op("dve", lambda e: e.reciprocal(out=sm[:, 10:12], in_=sm[:, 8:10]), r=[tk], w=[tk])

            def m9():
                for hh in range(2):
                    TS("dve", pnb[k][:, hh * 256:(hh + 1) * 256], pb[k][:, hh * 256:(hh + 1) * 256],
                       sm[:, 10 + hh:11 + hh], None, ALU.mult, ALU.bypass, [tpb[k], tk], [tpnb[k]])

            def m10():
                pT = bank(6).bitcast(BF16)[:, 0:512]
                for q4 in range(4):
                    P.op("pe", lambda e, q4=q4: e.transpose(out=pT[:, q4 * 128:(q4 + 1) * 128],
                                                             in_=pnb[k][:, q4 * 128:(q4 + 1) * 128], identity=ident),
                         r=[tpnb[k]], w=[TB[6]])

            def m11():
                pT = bank(6).bitcast(BF16)[:, 0:512]
                P.op("dve", lambda e: e.tensor_copy(out=pTb[k], in_=pT), r=[TB[6]], w=[tpTb[k]])

            def m12():
                oc = bank(7)[:, gi * 128:(gi + 1) * 128]
                for q4 in range(4):
                    kv, kt = q4 // 2, q4 % 2
                    MM(oc, Vp[s][kv][:, kt, :], pTb[k][:, q4 * 128:(q4 + 1) * 128], q4 == 0, q4 == 3,
                       [tVp[s], tpTb[k]], [TB[7]])

            def m13():
                if gi == 3:
                    ACT(OswSt[s][:, half * 512:(half + 1) * 512], bank(7), AF.Copy, [TB[7]], [tOswSt[s]])
                    if half == 1:
                        DMA("pool", OTsw[j], OswSt[s], [tOswSt[s]], [tOTsw[j]], "COsw%d" % s)

            return [m0, m1, m2, m3, m4, m5, m6, m7, m8, m9, m10, m11, m12, m13]

        swa_all = [swa_micro(j, g, j * 8 + g) for j in range(NJ) for g in range(8)]
        NMS = 14
        SW_SP = 4
        swa_T = [0]
        SWA_TICKS = SW_SP * (len(swa_all) - 1) + NMS

        def swa_tick():
            T = swa_T[0]
            swa_T[0] += 1
            if T >= SWA_TICKS:
                return
            G0 = max(0, (T - NMS + SW_SP) // SW_SP)
            for G in range(G0, min(len(swa_all), T // SW_SP + 1)):
                m = T - SW_SP * G
                if 0 <= m < NMS:
                    swa_all[G][m]()

        its = []
        for j in range(NJ):
            for gq in range(j, -1, -1):
                for tp in (1, 0):
                    its.append((j, gq, tp))
        NIT = len(its)
        slot_of = {}
        nload = [0]
        Sbuf = {}
        NB4 = 4
        psZ = PP[0]
        psC = PP[1]
        tZ = [TB[0], TB[1]]
        tCb = [TB[2], TB[3]]

        def stageZ1(n):
            j, gq, tp = its[n]
            first = (gq == j and tp == 1)
            js = j % 2
            if first:
                if j == 0:
                    DMA("sp", Qsb[0], QTsb[0], [tQTsb[0]], [tQsb[0]], "CQsb0")
                if j + 1 < NJ:
                    jn = (j + 1) % 2
                    DMA("sp", Qsb[jn], QTsb[j + 1], [tQTsb[j + 1]], [tQsb[jn]], "CQsb%d" % jn)
            if tp == 1:
                sl = nload[0] % NKV
                nload[0] += 1
                slot_of[(j, gq)] = sl
                DMA("sp", KTg[sl].rearrange("p h t -> p (h t)"), KTs[gq], [tKTs[gq]], [tKTg[sl]], "CKT%d" % sl)
                DMA("sp", Vg[sl].rearrange("p t n -> p (t n)"), Vs[gq], [tVs[gq]], [tVg[sl]], "CV%d" % sl)
            sl = slot_of[(j, gq)]
            es = n % NB4
            for u in range(2):
                t = 2 * tp + 1 - u
                for h in range(4):
                    MM(psZ[:, u * 512 + h * 128:u * 512 + (h + 1) * 128], KTg[sl][:, h, t * 128:(t + 1) * 128],
                       Qsb[js][:, h * 128:(h + 1) * 128], True, True, [tKTg[sl], tQsb[js]], tZ)
            ACT(eb[es], psZ[:, :], AF.Exp, tZ, [teb[es]], scale=float(SCALE_SB))
            if gq == j:
                for u in range(2):
                    t = 2 * tp + 1 - u
                    e3 = eb[es][:, u * 512:(u + 1) * 512].rearrange("p (h q) -> p h q", h=4)
                    TT("dve", e3, e3, Msb[:, t, :].unsqueeze(1).to_broadcast([128, 4, 128]), ALU.mult, [teb[es], tM], [teb[es]])

        def stageZ2(n):
            j, gq, tp = its[n]
            first = (gq == j and tp == 1)
            last = (gq == 0 and tp == 0)
            es = n % NB4
            ACT(spb[es], eb[es], AF.Ln, [teb[es]], [tspb[es]], bias=1.0)
            if not last:
                a = n % NB4
                sp_hi = spb[es][:, 0:512]
                sp_lo = spb[es][:, 512:1024]
                if first:
                    TT("dve", accb[a], sp_hi, sp_lo, ALU.add, [tspb[es]], [taccb[a]])
                else:
                    pa, pt = Sbuf[n - 1]
                    TT("dve", sumb[a], sp_hi, sp_lo, ALU.add, [tspb[es]], [tsumb[a]])
                    TT("dve", accb[a], pa, sumb[a], ALU.add, [pt, tsumb[a]], [taccb[a]])
                Sbuf[n] = (accb[a], taccb[a])

        def stageC1(n):
            j, gq, tp = its[n]
            first = (gq == j and tp == 1)
            es = n % NB4
            sp_hi = spb[es][:, 0:512]
            sp_lo = spb[es][:, 512:1024]
            hi = psC[:, 0:512]
            lo = psC[:, 512:1024]
            MM(hi, uincl, sp_hi, True, first, [tspb[es]], tCb)
            if not first:
                pa, pt = Sbuf[n - 1]
                MM(hi, ones, pa, False, True, [pt], tCb)
            MM(lo, uincl, sp_lo, True, False, [tspb[es]], tCb)
            MM(lo, ones, sp_hi, False, first, [tspb[es]], tCb)
            if not first:
                MM(lo, ones, pa, False, True, [pt], tCb)
            k = n % 2
            ACT(E2b[k], psC[:, :], AF.Exp, tCb, [tE2b[k]], scale=-1.0)

        def stageC2(n):
            es = n % NB4
            k = n % 2
            TT("dve", aTb[es], eb[es], E2b[k], ALU.mult, [teb[es], tE2b[k]], [taTb[es]])

        def stageAV(n):
            j, gq, tp = its[n]
            first = (gq == j and tp == 1)
            last = (gq == 0 and tp == 0)
            sl = slot_of[(j, gq)]
            es = n % NB4
            js = j % 2
            for u in range(2):
                t = 2 * tp + 1 - u
                for h in range(4):
                    MM(bank(4)[:, h * 128:(h + 1) * 128], Vg[sl][:, t, h * 128:(h + 1) * 128],
                       aTb[es][:, u * 512 + h * 128:u * 512 + (h + 1) * 128],
                       first and u == 0 and h == 0, last and u == 1, [tVg[sl], taTb[es]], [TB[4]])
            if last:
                P.op("dve", lambda e, js=js: e.tensor_copy(out=OsbSt[js], in_=bank(4)), r=[TB[4]], w=[tOsbSt[js]])
                DMA("pool", OTsb[j], OsbSt[js], [tOsbSt[js]], [tOTsb[j]], "COsb%d" % js)

        SKA, SKB = 2, 4
        for n in range(NIT + SKB):
            if n < NIT:
                stageZ1(n)
            if SKA <= n < NIT + SKA:
                stageC1(n - SKA)
            if n < NIT:
                stageZ2(n)
            if SKA <= n < NIT + SKA:
                stageC2(n - SKA)
            if n >= SKB:
                stageAV(n - SKB)
            swa_tick()
        while swa_T[0] < SWA_TICKS:
            swa_tick()

        P.barrier()
        AR.release(base_mark)
        if stop_after == "C":
            return finish(nc, P, st, out, None)

        WO = AR.alloc(16 * D, BF16).rearrange("p (c n) -> p c n", c=16)
        WRs = AR.alloc(16 * 36, BF16).rearrange("p (c n) -> p c n", c=16)
        tWD = Tok()
        wO3 = wOut.rearrange("(c p) n -> p c n", p=128)
        for c0 in range(0, 16, 4):
            DMA("pool", WO[:, c0:c0 + 4, :], wO3[:, c0:c0 + 4, :], [], [tWD], "DW")
        DMA("pool", WRs, wR.rearrange("(c p) n -> p c n", p=128), [], [tWD], "DW")
        bRb = AR.alloc(36, F32)
        erowb = AR.alloc(NE, F32)
        DMA("sp", bRb, bR.to_broadcast([128, 36]), [], [tWD], "Dc")
        DMA("sp", erowb, erow.to_broadcast([128, NE]), [], [tWD], "Dc")
        nD = make_norm_ctx(nffn, "D", sep_junk=True)
        OTsbg = [AR.alloc(4 * 512, BF16).rearrange("p (c t) -> p c t", c=4) for _ in range(2)]
        OTswg = [AR.alloc(8 * 512, BF16).rearrange("p (c t) -> p c t", c=8) for _ in range(2)]
        hTog1 = AR.alloc(16 * 512, BF16).rearrange("p (c t) -> p c t", c=16)
        hTog = [hTog1, hTog1]
        tIn = [[Tok(), Tok()] for _ in range(2)]
        tInH = Tok()
        for s_ in range(2):
            tIn[s_].append(tInH)
        WFs = [AR.alloc(5632, BF16) for _ in range(2)]
        tWGp = [[Tok() for _ in range(3)] for _ in range(2)]
        mT = AR.alloc(16 * 512, BF16).rearrange("p (c t) -> p c t", c=16)
        tmT = [Tok() for _ in range(16)]
        sg = [AR.alloc(512, BF16) for _ in range(2)]
        tsg = [Tok() for _ in range(2)]
        tt1 = AR.alloc(512, F32)
        ttt1 = Tok()
        tt2 = AR.alloc(512, F32)
        ttt2 = Tok()
        h2T = [AR.alloc(16 * 128, BF16).rearrange("p (c t) -> p c t", c=16) for _ in range(2)]
        th2T = [Tok() for _ in range(2)]
        rt = [AR.alloc(256, F32) for _ in range(2)]
        trt = [Tok() for _ in range(2)]
        selb = [AR.alloc(NE, BF16) for _ in range(2)]
        tselb = [Tok() for _ in range(2)]
        selacc = [AR.alloc(NE, BF16) for _ in range(2)]
        tselacc = [Tok() for _ in range(2)]
        tX1 = [Tok() for _ in range(NJ)]
        tXS = Tok()
        tIdx = Tok()
        nwg = [0]
        for tg in range(NJ // 4):
            s = tg % 2
            for jj in range(4):
                j = 4 * tg + jj
                DMA("sp", OTsbg[s][:, :, jj * 128:(jj + 1) * 128], OTsb[j].rearrange("p (h q) -> p h q", h=4), [tOTsb[j]], [tIn[s][0]], "DI0%d" % s)
                DMA("sp", OTswg[s][:, :, jj * 128:(jj + 1) * 128], OTsw[j].rearrange("p (g q) -> p g q", g=8), [tOTsw[j]], [tIn[s][1]], "DI1%d" % s)
                DMA("sp", hTog[s][:, :, jj * 128:(jj + 1) * 128], HT[j].rearrange("p (c t) -> p c t", c=16), [tHT[j]], [tIn[s][2]], "DI2")
            for f in range(16):
                ws = nwg[0] % 2
                bo = 4 * (f % 2)
                nwg[0] += 1
                for (c0, c1) in ((0, 2048), (2048, 4096), (4096, 5632)):
                    DMA("sp", WFs[ws][:, c0:c1], WFb[f][:, c0:c1], [], [tWGp[ws][c0 // 2048]], "DWG%d_%d" % (ws, c0))
                WGv = WFs[ws][:, 0:4096].rearrange("p (c n) -> p c n", c=16)
                WUv = WFs[ws][:, 4096:5632].rearrange("p (c n) -> p c n", c=12)
                for kc in range(4):
                    MM(bank(bo + 0), WUv[:, kc, :], OTsbg[s][:, kc, :], kc == 0, kc == 3, [tWGp[ws][2], tIn[s][0]], [TB[bo + 0]])
                for kc in range(8):
                    MM(bank(bo + 1), WUv[:, 4 + kc, :], OTswg[s][:, kc, :], kc == 0, kc == 7, [tWGp[ws][2], tIn[s][1]], [TB[bo + 1]])
                for gsel in range(2):
                    for cc in range(16):
                        MM(bank(bo + 2 + gsel), WGv[:, cc, gsel * 128:(gsel + 1) * 128], hTog[s][:, cc, :], cc == 0, cc == 15,
                           [tWGp[ws][cc // 8], tIn[s][2]], [TB[bo + 2 + gsel]])
                ACT(sg[0], bank(bo + 2), AF.Sigmoid, [TB[bo + 2]], [tsg[0]])
                ACT(sg[1], bank(bo + 3), AF.Sigmoid, [TB[bo + 3]], [tsg[1]])
                TT("dve", tt1, sg[0], bank(bo + 0), ALU.mult, [tsg[0], TB[bo + 0]], [ttt1])
                TT("dve", tt2, sg[1], bank(bo + 1), ALU.mult, [tsg[1], TB[bo + 1]], [ttt2])
                TT("pool", mT[:, f, :], tt1, tt2, ALU.add, [ttt1, ttt2], [tmT[f]])
            def part1a(jj, tg=tg):
                j = 4 * tg + jj
                xs_ = nD.n % 2
                nD.n += 1
                xtile = nD.xt[xs_]
                txt = nD.txt[xs_]
                DMA("sp", xtile, xo[j], [], [txt], "Dx%d" % xs_)
                for nn in range(4):
                    bx = 4 + nn % 2
                    for f in range(16):
                        MM(bank(bx), mT[:, f, jj * 128:(jj + 1) * 128], WO[:, f, nn * 512:(nn + 1) * 512], f == 0, f == 15,
                           [tmT[f], tWD], [TB[bx]])
                    TT("dve", xtile[:, nn * 512:(nn + 1) * 512], bank(bx), xtile[:, nn * 512:(nn + 1) * 512], ALU.add,
                       [TB[bx], txt], [txt])
                DMA("pool", X1[j], xtile, [txt], [tX1[j]], "Dx%d" % xs_)
                norm_stats(nD, xs_, xtile, txt)
                return dict(j=j, xs_=xs_, xtile=xtile, txt=txt)

            def part1b1(sta):
                j, xs_, xtile, txt = sta['j'], sta['xs_'], sta['xtile'], sta['txt']
                h2 = nD.xn[xs_]
                th2 = nD.txn[xs_]
                STT(h2, xtile, nD.st[xs_][:, 0:1], nD.gbc, ALU.mult, ALU.mult, [txt, nD.tst[xs_], nD.tg], [th2])

            def part1b(sta):
                j, xs_, xtile, txt = sta['j'], sta['xs_'], sta['xtile'], sta['txt']
                h2 = nD.xn[xs_]
                th2 = nD.txn[xs_]
                hs = j % 2
                transpose_tile(h2, th2, 3, h2T[hs], th2T[hs], "act")
                br_ = 4 + j % 2
                for cc in range(16):
                    MM(bank(br_)[:, 0:36], h2T[hs][:, cc, :], WRs[:, cc, :], cc == 0, cc == 15, [th2T[hs], tWD], [TB[br_]])
                R = rt[hs]
                tR = trt[hs]
                lg = R[:, 0:36]
                TT("dve", lg, bank(br_)[:, 0:36], bRb, ALU.add, [TB[br_], tWD], [tR])
                return dict(j=j, hs=hs, R=R, tR=tR, lg=lg, h2=h2, th2=th2, xs_=xs_)

            def part2(stt):
                j, hs, R, tR, lg, h2, th2, xs_ = (stt[k_] for k_ in ('j', 'hs', 'R', 'tR', 'lg', 'h2', 'th2', 'xs_'))
                rb = j % 2
                gmax = R[:, 36:37]
                P.op("dve", lambda e, lg=lg, gmax=gmax: e.tensor_reduce(out=gmax, in_=lg[:, 0:4], axis=AX.X, op=ALU.max), r=[tR], w=[tR])
                gm = R[:, 40:44]
                TS("dve", gm, lg[:, 0:4], gmax, None, ALU.is_ge, ALU.bypass, [tR], [tR])
                gd = R[:, 44:48]
                TS("dve", gd, lg[:, 0:4], gmax, None, ALU.subtract, ALU.bypass, [tR], [tR])
                gsum = R[:, 37:38]
                ACT(gd, gd, AF.Exp, [tR], [tR], accum_out=gsum)
                pgrp = R[:, 38:39]
                P.op("dve", lambda e, pgrp=pgrp, gsum=gsum: e.reciprocal(out=pgrp, in_=gsum), r=[tR], w=[tR])
                pen = R[:, 48:52]
                TS("dve", pen, gm, BIG, -BIG, ALU.mult, ALU.add, [tR], [tR])
                ml = R[:, 64:96]
                TT("dve", ml.rearrange("p (g k) -> p g k", g=4), lg[:, 4:36].rearrange("p (g k) -> p g k", g=4),
                   pen.unsqueeze(2).to_broadcast([128, 4, 8]), ALU.add, [tR], [tR])
                m1 = R[:, 52:53]
                P.op("dve", lambda e, ml=ml, m1=m1: e.tensor_reduce(out=m1, in_=ml, axis=AX.X, op=ALU.max), r=[tR], w=[tR])
                is1 = R[:, 96:128]
                TS("dve", is1, ml, m1, None, ALU.is_ge, ALU.bypass, [tR], [tR])
                ml2 = R[:, 128:160]
                STT(ml2, is1, -BIG, ml, ALU.mult, ALU.add, [tR], [tR])
                m2 = R[:, 53:54]
                P.op("dve", lambda e, ml2=ml2, m2=m2: e.tensor_reduce(out=m2, in_=ml2, axis=AX.X, op=ALU.max), r=[tR], w=[tR])
                is2 = R[:, 160:192]
                TS("dve", is2, ml2, m2, None, ALU.is_ge, ALU.bypass, [tR], [tR])
                d21 = R[:, 54:55]
                TT("dve", d21, m2, m1, ALU.subtract, [tR], [tR])
                e21 = R[:, 55:56]
                ACT(e21, d21, AF.Exp, [tR], [tR])
                den = R[:, 56:57]
                TS("dve", den, e21, 1.0, None, ALU.add, ALU.bypass, [tR], [tR])
                w1 = R[:, 57:58]
                P.op("dve", lambda e, w1=w1, den=den: e.reciprocal(out=w1, in_=den), r=[tR], w=[tR])
                TT("dve", gt1[:, j:j + 1], w1, pgrp, ALU.mult, [tR], [tIdx])
                TT("dve", gt2[:, j:j + 1], gt1[:, j:j + 1], e21, ALU.mult, [tR, tIdx], [tIdx])
                TT("dve", selb[hs], is1, is2, ALU.add, [tR], [tselb[hs]])
                MM(bank(rb)[:, 0:32], ustrict, selb[hs], True, j == 0, [tselb[hs]], [TB[rb]])
                if j > 0:
                    MM(bank(rb)[:, 0:32], ones, selacc[(j - 1) % 2], False, True, [tselacc[(j - 1) % 2]], [TB[rb]])
                if j == 0:
                    P.op("pool", lambda e, hs=hs: e.tensor_copy(out=selacc[0], in_=selb[hs]), r=[tselb[hs]], w=[tselacc[0]])
                else:
                    TT("pool", selacc[j % 2], selacc[(j - 1) % 2], selb[hs], ALU.add, [tselacc[(j - 1) % 2], tselb[hs]], [tselacc[j % 2]])
                dest = R[:, 192:224]
                TT("dve", dest, bank(rb)[:, 0:32], erowb, ALU.add, [TB[rb], tWD], [tR])
                i1f = R[:, 58:59]
                i2f = R[:, 59:60]
                junk32 = R[:, 224:256]
                STT(junk32, is1, 1.0, dest, ALU.mult, ALU.mult, [tR], [tR], accum_out=i1f)
                STT(junk32, is2, 1.0, dest, ALU.mult, ALU.mult, [tR], [tR], accum_out=i2f)
                P.op("dve", lambda e, j=j, i1f=i1f: e.tensor_copy(out=idx1[:, j:j + 1], in_=i1f), r=[tR], w=[tIdx])
                P.op("dve", lambda e, j=j, i2f=i2f: e.tensor_copy(out=idx2[:, j:j + 1], in_=i2f), r=[tR], w=[tIdx])
                for ix in (idx1, idx2):
                    P.op("pool", lambda e, ix=ix, j=j, h2=h2: e.indirect_dma_start(
                        out=XS, out_offset=bass.IndirectOffsetOnAxis(ap=ix[:, j:j + 1], axis=0),
                        in_=h2, in_offset=None, bounds_check=bc_reg(e), oob_is_err=False),
                        r=[th2, tIdx], w=[], key="Dsc%d" % xs_)


            sa_ = [part1a(0)]
            sb_ = []
            for jj in range(4):
                part1b1(sa_[jj])
                if jj + 1 < 4:
                    sa_.append(part1a(jj + 1))
                sb_.append(part1b(sa_[jj]))
                if jj >= 1:
                    part2(sb_[jj - 1])
            part2(sb_[3])

        if dbg and "RT" in dbg:
            dI1 = nc.dram_tensor("dI1", [128, NJ], I32, kind="ExternalOutput").ap()
            dI2 = nc.dram_tensor("dI2", [128, NJ], I32, kind="ExternalOutput").ap()
            dG1 = nc.dram_tensor("dG1", [128, NJ], F32, kind="ExternalOutput").ap()
            dG2 = nc.dram_tensor("dG2", [128, NJ], F32, kind="ExternalOutput").ap()
            for dd, ss in ((dI1, idx1), (dI2, idx2), (dG1, gt1), (dG2, gt2)):
                DMA("sp", dd, ss, [tIdx], [], "Dc")
        print('arena D high', AR.off, AR.n)
        P.barrier()
        AR.release(base_mark)
        if stop_after == "D":
            return finish(nc, P, st, out, None)

        Wg_ = [AR.alloc(16 * 512, BF16).rearrange("p (c n) -> p c n", c=16) for _ in range(2)]
        Wu_ = [AR.alloc(16 * 512, BF16).rearrange("p (c n) -> p c n", c=16) for _ in range(2)]
        Wd_ = [AR.alloc(4 * D, BF16).rearrange("p (c n) -> p c n", c=4) for _ in range(2)]
        tWgp = [[Tok() for _ in range(4)] for _ in range(2)]
        tWup = [[Tok() for _ in range(4)] for _ in range(2)]
        tWdp = [[Tok() for _ in range(2)] for _ in range(2)]
        xsr = [AR.alloc(D, BF16) for _ in range(4)]
        txsr = [Tok() for _ in range(4)]
        xsT = [AR.alloc(16 * CAP, BF16).rearrange("p (c t) -> p c t", c=16) for _ in range(2)]
        txsT = [[Tok() for _ in range(NCT)] for _ in range(2)]
        sa = [AR.alloc(CAP, F32) for _ in range(2)]
        tsa = [Tok() for _ in range(2)]
        hTe = AR.alloc(4 * CAP, BF16).rearrange("p (c t) -> p c t", c=4)
        thTe = [Tok() for _ in range(4)]
        ysb = [AR.alloc(D, F32) for _ in range(2)]
        tysb = [Tok() for _ in range(2)]
        tYS = Tok()
        nxs = [0]
        nys = [0]
        def e_weights(ex):
            s = ex % 2
            wg3 = wGate[ex].rearrange("(c p) n -> p c n", p=128)
            wu3 = wUp[ex].rearrange("(c p) n -> p c n", p=128)
            wd3 = wDown[ex].rearrange("(c p) n -> p c n", p=128)
            for c0 in range(0, 16, 4):
                DMA("pool", Wg_[s][:, c0:c0 + 4, :], wg3[:, c0:c0 + 4, :], [], [tWgp[s][c0 // 4]], "EWg%d_%d" % (s, c0))
            for c0 in range(0, 16, 4):
                DMA("pool", Wu_[s][:, c0:c0 + 4, :], wu3[:, c0:c0 + 4, :], [], [tWup[s][c0 // 4]], "EWu%d_%d" % (s, c0))
            for c0 in range(0, 4, 2):
                DMA("pool", Wd_[s][:, c0:c0 + 2, :], wd3[:, c0:c0 + 2, :], [], [tWdp[s][c0 // 2]], "EWd%d_%d" % (s, c0))

        def e_rows(ex, tt):
            s = ex % 2
            k = nxs[0] % 4
            nxs[0] += 1
            DMA("sp", xsr[k], XS[ex * CAP + tt * 128:ex * CAP + (tt + 1) * 128, :], [], [txsr[k]], "Exs%d" % k)
            transpose_tile(xsr[k], txsr[k], 3, xsT[s][:, :, tt * 128:(tt + 1) * 128], txsT[s][tt], "dve" if tt % 2 else "act")

        e_weights(0)
        for tt in range(NCT):
            e_rows(0, tt)
        YB = [4, 5, 0, 1]
        for ex in range(NE):
            s = ex % 2
            if ex + 1 < NE:
                e_weights(ex + 1)
            for dc in range(4):
                for cc in range(16):
                    MM(bank(0 + dc % 2)[:, 0:CAP], Wg_[s][:, cc, dc * 128:(dc + 1) * 128], xsT[s][:, cc, :], cc == 0, cc == 15,
                       [tWgp[s][cc // 4]] + txsT[s], [TB[0 + dc % 2]])
                for cc in range(16):
                    MM(bank(2 + dc % 2)[:, 0:CAP], Wu_[s][:, cc, dc * 128:(dc + 1) * 128], xsT[s][:, cc, :], cc == 0, cc == 15,
                       [tWup[s][cc // 4]] + txsT[s], [TB[2 + dc % 2]])
                ACT(sa[dc % 2], bank(0 + dc % 2)[:, 0:CAP], AF.Silu, [TB[0 + dc % 2]], [tsa[dc % 2]])
                TT("dve", hTe[:, dc, :], sa[dc % 2], bank(2 + dc % 2)[:, 0:CAP], ALU.mult, [tsa[dc % 2], TB[2 + dc % 2]], [thTe[dc]])
            for tt in range(NCT):
                k = nys[0] % 2
                nys[0] += 1
                for nn in range(4):
                    by = YB[nn]
                    for dc in range(4):
                        MM(bank(by), hTe[:, dc, tt * 128:(tt + 1) * 128], Wd_[s][:, dc, nn * 512:(nn + 1) * 512], dc == 0, dc == 3,
                           [thTe[dc], tWdp[s][dc // 2]], [TB[by]])
                    if nn % 2 == 0:
                        ACT(ysb[k][:, nn * 512:(nn + 1) * 512], bank(by), AF.Copy, [TB[by]], [tysb[k]])
                    else:
                        P.op("dve", lambda e, k=k, nn=nn, by=by: e.tensor_copy(out=ysb[k][:, nn * 512:(nn + 1) * 512], in_=bank(by)),
                             r=[TB[by]], w=[tysb[k]])
                DMA("act", YS[ex * CAP + tt * 128:ex * CAP + (tt + 1) * 128, :], ysb[k], [tysb[k]], [], "Eys%d" % k)
                if ex + 1 < NE:
                    e_rows(ex + 1, tt)

        P.barrier()
        AR.release(base_mark)

        nF = make_norm_ctx(nfin, "F", nslots=4)
        y1 = [AR.alloc(D, F32) for _ in range(4)]
        y2 = [AR.alloc(D, F32) for _ in range(4)]
        ty = [[Tok() for _ in range(2)] for _ in range(4)]
        ob = [AR.alloc(D, F32) for _ in range(2)]
        tob = [Tok() for _ in range(2)]
        tOut = [Tok() for _ in range(NJ)]
        def f_loads(j):
            s = j % 4
            DMA("sp", nF.xt[s], X1[j], [tX1[j]], [nF.txt[s]], "Fx%d" % s)
            for yy, ix, tk, kn in ((y1, idx1, ty[s][0], "Fy1%d" % s), (y2, idx2, ty[s][1], "Fy2%d" % s)):
                P.op("pool", lambda e, yy=yy, ix=ix, j=j, s=s: e.indirect_dma_start(
                    out=yy[s], out_offset=None, in_=YS,
                    in_offset=bass.IndirectOffsetOnAxis(ap=ix[:, j:j + 1], axis=0),
                    bounds_check=bc_reg(e), oob_is_err=False),
                    r=[tIdx], w=[tk], key=kn)

        for j in range(3):
            f_loads(j)
        for j in range(NJ):
            s = j % 4
            so = j % 2
            if j + 3 < NJ:
                f_loads(j + 3)
            xt_ = nF.xt[s]
            STT(xt_, y1[s], gt1[:, j:j + 1], xt_, ALU.mult, ALU.add, [ty[s][0], nF.txt[s], tIdx], [nF.txt[s]])
            STT(xt_, y2[s], gt2[:, j:j + 1], xt_, ALU.mult, ALU.add, [ty[s][1], nF.txt[s], tIdx], [nF.txt[s]])
            norm_stats(nF, s, xt_, nF.txt[s])
            STT(ob[so], xt_, nF.st[s][:, 0:1], nF.gbc, ALU.mult, ALU.mult, [nF.txt[s], nF.tst[s], nF.tg], [tob[so]])
            DMA("sp", out[j], ob[so], [tob[so]], [tOut[j]], "Fo%d" % so)

        return finish(nc, P, st, out, tOut)


def finish(nc, P, st, out, tOut):
    if tOut is not None:
        P.op("sp", None, r=tOut)
    else:
        P.barrier()
        P.op("sp", None)
    P.emit(st)
    print('ops', len(P.ops), 'waits', P.n_waits, 'keys', len(P.key_cnt))
    return nc


def _layouts(inp):
    f32 = np.float32
    w_in = np.asarray(inp["w_in"])[0]
    qsw_src = np.arange(1024).reshape(2, 8, 64).transpose(1, 0, 2).reshape(-1)
    colsA = np.concatenate([np.arange(512, 1024), np.arange(1024, 1536), np.arange(0, 512),
                            1536 + qsw_src, np.arange(2560, 2688), np.arange(2688, 2816)])
    wA = np.ascontiguousarray(w_in[:, colsA])
    wUsw = np.asarray(inp["w_up_sw"])[0][qsw_src, :]
    wUsb = np.asarray(inp["w_up_sb"])[0]
    wF = np.empty((16, 128, 5632), f32)
    wF[:, :, 0:4096] = w_in[:, 2816:].reshape(16, 128, 2, 16, 128).transpose(3, 1, 0, 2, 4).reshape(16, 128, 4096)
    wF[:, :, 4096:4608] = wUsb.reshape(4, 128, 16, 128).transpose(2, 1, 0, 3).reshape(16, 128, 512)
    wF[:, :, 4608:5632] = wUsw.reshape(8, 128, 16, 128).transpose(2, 1, 0, 3).reshape(16, 128, 1024)
    sinks = np.asarray(inp["sinks"])[0]
    sinks_p = np.ascontiguousarray(sinks.reshape(2, 8).T.reshape(1, 16))
    wR = np.ascontiguousarray(np.concatenate([np.asarray(inp["w_router_group"])[0], np.asarray(inp["w_router_expert"])[0]], axis=1))
    bR = np.ascontiguousarray(np.concatenate([np.asarray(inp["b_router_group"])[0], np.asarray(inp["b_router_expert"])[0]])[None, :])
    common = {
        "wA": wA, "wF": wF,
        "wOut": np.ascontiguousarray(np.asarray(inp["w_out"])[0]),
        "wR": wR, "bR": bR,
        "wGate": np.ascontiguousarray(np.asarray(inp["w_gate"])[0]),
        "wUp": np.ascontiguousarray(np.asarray(inp["w_up"])[0]),
        "wDown": np.ascontiguousarray(np.asarray(inp["w_down"])[0]),
        "nmix": np.ascontiguousarray(np.asarray(inp["norm_mix"]).reshape(1, D)),
        "nffn": np.ascontiguousarray(np.asarray(inp["norm_ffn"]).reshape(1, D)),
        "nfin": np.ascontiguousarray(np.asarray(inp["norm_final"]).reshape(1, D)),
        "sinks": sinks_p,
        "erow": (np.arange(NE, dtype=f32) * CAP)[None, :],
    }
    kk = np.arange(128)[:, None]
    qq = np.arange(128)[None, :]
    tri = (kk < qq).astype(f32)
    slopes = np.exp2(-8.0 * np.arange(1, 17, dtype=np.float64) / 16.0)
    qi = np.arange(128)[:, None]
    kj = np.arange(256)[None, :]
    dist = qi + 128 - kj
    valid = (dist >= 0) & (dist < 128)
    hp = np.arange(16).reshape(2, 8).T.reshape(-1)
    bias_gen = np.where(valid[:, None, :], -slopes[hp][None, :, None] * dist[:, None, :], -1e30).astype(f32)
    bias_first = bias_gen.copy()
    bias_first[:, :, 0:128] = -1e30
    x = np.asarray(inp["x"])
    maps = []
    for c in range(8):
        b, r = c // 4, c % 4
        xbv = x[b].reshape(128, 128, D)
        x4 = x[b].reshape(32, 4, 128, D)
        xo = np.ascontiguousarray(x4[:, r])
        if r > 0:
            xp = np.ascontiguousarray(x4[:, r - 1])
        else:
            xp = np.zeros((32, 128, D), f32)
            xp[1:] = x4[:-1, 3]
        m = np.zeros((128, 4, 128), f32)
        for t in range(4):
            if t < r:
                m[:, t, :] = 1.0
            elif t == r:
                m[:, t, :] = tri
        swb = np.stack([bias_first if r == 0 else bias_gen, bias_gen]).astype(f32)
        d = dict(common)
        d.update({"xb": xbv, "xo": xo, "xp": xp, "sbmask": m, "swbias": swb})
        maps.append(d)
    return maps


_NC_CACHE = {}


def kernel(**inputs):
    maps = _layouts(inputs)
    if "nc" not in _NC_CACHE:
        _NC_CACHE["nc"] = build_nc()
    nc = _NC_CACHE["nc"]
    res = run_bass_kernel_spmd(nc, maps, core_ids=list(range(8)))
    outp = np.empty((2, S, D), np.float32)
    o5 = outp.reshape(2, 32, 4, 128, D)
    for c in range(8):
        b, r = c // 4, c % 4
        o5[b, :, r] = np.asarray(res.results[c]["out"]).reshape(32, 128, D)
    return outp
```

```python
import contextlib
import numpy as np
import concourse.bass as bass
import concourse.mybir as mybir
from concourse.bass_utils import run_bass_kernel_spmd

F32 = mybir.dt.float32
BF16 = mybir.dt.bfloat16
I32 = mybir.dt.int32
AF = mybir.ActivationFunctionType
ALU = mybir.AluOpType
AX = mybir.AxisListType

ENGS = ("sp", "act", "pool", "dve", "pe")

D = 2048
S = 16384
NJ = 32
NG = 32
CAP = 384
NCT = CAP // 128
NE = 32
EPS = 1e-6
BIG = 1.0e4


class Tok:
    __slots__ = ("w", "rs")

    def __init__(self):
        self.w = None
        self.rs = {}


class Op:
    __slots__ = ("eng", "fn", "deps", "dma", "key", "tick", "semval", "has_dep", "idx")


class Prog:
    def __init__(self, nc):
        self.nc = nc
        self.ops = []
        self.key_last = {}
        self.key_cnt = {}
        self.last_on_eng = {}
        self.pending_barrier = {}
        self.kmap = {}

    def op(self, eng, fn, r=(), w=(), key=None):
        if key is not None:
            key = self.kmap.setdefault(key, len(self.kmap))
        o = Op()
        o.idx = len(self.ops)
        o.eng = eng
        o.fn = fn
        o.dma = key is not None
        o.key = key
        o.tick = None
        o.semval = None
        o.has_dep = False
        deps = set()
        for t in r:
            if t.w is not None:
                deps.add(t.w)
        for t in w:
            if t.w is not None:
                deps.add(t.w)
            deps.update(t.rs.values())
        if o.dma:
            if key in self.key_last:
                deps.add(self.key_last[key])
            self.key_last[key] = o.idx
            self.key_cnt[key] = self.key_cnt.get(key, 0) + 1
            o.semval = 16 * self.key_cnt[key]
        if eng in self.pending_barrier:
            deps.update(self.pending_barrier.pop(eng))
        deps.discard(o.idx)
        o.deps = deps
        rk = ("dma", o.idx) if o.dma else eng
        for t in r:
            t.rs[rk] = o.idx
        for t in w:
            t.w = o.idx
            t.rs = {}
        self.ops.append(o)
        if not o.dma:
            self.last_on_eng[eng] = o.idx
        return o

    def barrier(self):
        deps = set(self.last_on_eng.values()) | set(self.key_last.values())
        for e in ENGS:
            self.pending_barrier.setdefault(e, set()).update(deps)
        self.kmap = {}

    def emit(self, stack):
        nc = self.nc
        ops = self.ops
        for o in ops:
            for d in o.deps:
                p = ops[d]
                if p.dma or (p.eng == "pe" and o.eng == "pe"):
                    continue
                p.has_dep = True
        cnt = {e: 0 for e in ENGS}
        for o in ops:
            if not o.dma and o.has_dep:
                cnt[o.eng] += 1
                o.tick = cnt[o.eng]
        esem = {e: stack.enter_context(nc.semaphore("s_" + e)) for e in ENGS}
        ksem = {}
        for k in self.key_cnt:
            ksem[k] = stack.enter_context(nc.semaphore("k%d" % len(ksem)))
        by_eng = {e: [o for o in ops if o.eng == e] for e in ENGS}
        self.n_waits = 0

        def run(ename, e):
            seen = {}
            for o in by_eng[ename]:
                need = {}
                for d in o.deps:
                    p = ops[d]
                    if p.dma:
                        s, v = ksem[p.key], p.semval
                    else:
                        if p.eng == "pe" and ename == "pe":
                            continue
                        s, v = esem[p.eng], p.tick
                    if v > need.get(id(s), (None, 0))[1]:
                        need[id(s)] = (s, v)
                for s, v in need.values():
                    if seen.get(id(s), 0) < v:
                        e.wait_ge(s, v)
                        seen[id(s)] = v
                        self.n_waits += 1
                if o.fn is None:
                    continue
                ins = o.fn(e)
                if o.dma:
                    ins.then_inc(ksem[o.key], 16)
                elif o.has_dep:
                    ins.then_inc(esem[ename], 1)

        with nc.Block() as block:

            @block.sync
            def _(e):
                run("sp", e)

            @block.scalar
            def _(e):
                run("act", e)

            @block.gpsimd
            def _(e):
                run("pool", e)

            @block.vector
            def _(e):
                run("dve", e)

            @block.tensor
            def _(e):
                run("pe", e)


class Arena:
    def __init__(self, t, nelem):
        self.t = t
        self.n = nelem
        self.off = 0

    def alloc(self, n_elems, dt):
        nb = n_elems * (2 if dt == BF16 else 4)
        nb = (nb + 63) // 64 * 64
        a = self.off
        self.off += nb // 2
        assert self.off <= self.n, ("arena overflow", self.off, self.n)
        v = self.t[:, a:a + (n_elems * (2 if dt == BF16 else 4)) // 2]
        if dt != BF16:
            v = v.bitcast(dt)
        return v

    def mark(self):
        return self.off

    def release(self, m):
        self.off = m


def build_nc(dbg=False, stop_after=None):
    nc = bass.Bass("TRN2", target_bir_lowering=False)

    def din(name, shape, dt=F32):
        return nc.dram_tensor(name, list(shape), dt, kind="ExternalInput").ap()

    xb = din("xb", [128, 128, D])
    xo = din("xo", [NJ, 128, D])
    xp = din("xp", [NJ, 128, D])
    wA = din("wA", [D, 2816])
    wF = din("wF", [16, 128, 5632])
    wOut = din("wOut", [D, D])
    wR = din("wR", [D, 36])
    bR = din("bR", [1, 36])
    wGate = din("wGate", [NE, D, 512])
    wUp = din("wUp", [NE, D, 512])
    wDown = din("wDown", [NE, 512, D])
    nmix = din("nmix", [1, D])
    nffn = din("nffn", [1, D])
    nfin = din("nfin", [1, D])
    sinks = din("sinks", [1, 16])
    sbmask = din("sbmask", [128, 4, 128])
    swbias = din("swbias", [2, 128, 16, 256])
    erow = din("erow", [1, NE])
    out = nc.dram_tensor("out", [NJ, 128, D], F32, kind="ExternalOutput").ap()

    def scratch(name, shape, dt):
        return nc.dram_tensor(name, list(shape), dt, kind=("ExternalOutput" if (dbg and name in dbg) else "Internal")).ap()

    KTs = scratch("KTs", [NG, 128, 4 * 512], BF16)
    Vs = scratch("Vs", [NG, 128, 4 * 512], BF16)
    HT = scratch("HT", [NJ, 128, 16 * 128], BF16)
    QTsb = scratch("QTsb", [NJ, 128, 512], BF16)
    QTsw = scratch("QTsw", [NJ, 128, 1024], BF16)
    KTsw = scratch("KTsw", [NJ, 128, 256], BF16)
    Vsw = scratch("Vsw", [NJ, 128, 256], BF16)
    OTsb = scratch("OTsb", [NJ, 128, 512], BF16)
    OTsw = scratch("OTsw", [NJ, 128, 1024], BF16)
    X1 = scratch("X1", [NJ, 128, D], F32)
    XS = scratch("XS", [NE * CAP, D], BF16)
    YS = scratch("YS", [NE * CAP, D], F32)
    WFb = scratch("WFb", [16, 128, 5632], BF16)

    st = contextlib.ExitStack()
    with st:
        ARN = 102 * 1024
        arena_t = st.enter_context(nc.sbuf_tensor("arena", [128, ARN], BF16))
        AR = Arena(arena_t, ARN)
        PP = [st.enter_context(nc.psum_tensor("pp%d" % i, [128, 1024], F32)) for i in range(4)]
        TB = [Tok() for _ in range(8)]

        def bank(i):
            return PP[i // 2][:, (i % 2) * 512:(i % 2 + 1) * 512]

        P = Prog(nc)
        bc_cache = {}

        def bc_reg(e):
            if "r" not in bc_cache:
                rg = e.alloc_register("bcreg")
                e.reg_mov(rg, NE * CAP - 1)
                bc_cache["r"] = rg
            return bc_cache["r"]

        def DMA(eng, out_, in_, r, w, key, **kw):
            P.op(eng, lambda e: e.dma_start(out=out_, in_=in_, **kw), r=r, w=w, key=key)

        def MM(out_, lhsT, rhs, start, stop, r, w):
            P.op("pe", lambda e: e.matmul(out_, lhsT=lhsT, rhs=rhs, start=start, stop=stop), r=r, w=w)

        def ACT(out_, in_, func, r, w, **kw):
            P.op("act", lambda e: e.activation(out=out_, in_=in_, func=func, **kw), r=r, w=w)

        def TT(eng, out_, in0, in1, op, r, w):
            P.op(eng, lambda e: e.tensor_tensor(out=out_, in0=in0, in1=in1, op=op), r=r, w=w)

        def TS(eng, out_, in0, s1, s2, op0, op1, r, w, **kw):
            P.op(eng, lambda e: e.tensor_scalar(out=out_, in0=in0, scalar1=s1, scalar2=s2, op0=op0, op1=op1, **kw), r=r, w=w)

        def STT(out_, in0, scalar, in1, op0, op1, r, w, **kw):
            P.op("dve", lambda e: e.scalar_tensor_tensor(out=out_, in0=in0, scalar=scalar, in1=in1, op0=op0, op1=op1, **kw), r=r, w=w)

        identf = AR.alloc(128, F32)
        ident = AR.alloc(128, BF16)
        uincl = AR.alloc(128, BF16)
        ustrict = AR.alloc(128, BF16)
        ones = AR.alloc(128, BF16)
        mhalf = AR.alloc(2, F32)
        idx1 = AR.alloc(NJ, I32)
        idx2 = AR.alloc(NJ, I32)
        gt1 = AR.alloc(NJ, F32)
        gt2 = AR.alloc(NJ, F32)
        tC = Tok()

        def tri(dst, pattern, cm, cmp):
            P.op("pool", lambda e: e.memset(identf, 1.0), w=[tC])
            P.op("pool", lambda e: e.affine_select(out=identf, in_=identf, pattern=pattern, compare_op=cmp,
                                                   fill=0.0, base=0, channel_multiplier=cm), r=[tC], w=[tC])
            P.op("pool", lambda e: e.tensor_copy(out=dst, in_=identf), r=[tC], w=[tC])

        tri(ident, [[-1, 128]], 1, ALU.is_equal)
        tri(uincl, [[-1, 128]], 1, ALU.is_ge)
        tri(ustrict, [[1, 128]], -1, ALU.is_gt)
        P.op("pool", lambda e: e.memset(ones, 1.0), w=[tC])
        P.op("pool", lambda e: e.memset(mhalf[:, 0:1], -0.5), w=[tC])
        P.barrier()
        base_mark = AR.mark()

        class NormCtx:
            pass

        def make_norm_ctx(gvec_dram, keypfx, nslots=2, sep_junk=False):
            c = NormCtx()
            c.gbc = AR.alloc(D, F32)
            c.tg = Tok()
            DMA("sp", c.gbc, gvec_dram.to_broadcast([128, D]), [], [c.tg], keypfx + "g")
            c.nslots = nslots
            c.xt = [AR.alloc(D, F32) for _ in range(nslots)]
            c.txt = [Tok() for _ in range(nslots)]
            c.xn = [AR.alloc(D, BF16) for _ in range(2)]
            c.txn = [Tok() for _ in range(2)]
            c.st = [AR.alloc(2, F32) for _ in range(nslots)]
            c.tst = [Tok() for _ in range(nslots)]
            c.junk = AR.alloc(D, BF16) if sep_junk else None
            c.tjunk = Tok()
            c.n = 0
            c.nx = 0
            c.key = keypfx
            return c

        def norm_stats(c, s, xsrc, tx):
            if c.junk is not None:
                jk, tj = c.junk, c.tjunk
            else:
                jk, tj = c.xn[s % 2], c.txn[s % 2]
            STT(jk, xsrc, 1.0, xsrc, ALU.mult, ALU.mult, [tx], [tj, c.tst[s]], accum_out=c.st[s][:, 1:2])
            TS("pool", c.st[s][:, 1:2], c.st[s][:, 1:2], 1.0 / D, EPS, ALU.mult, ALU.add, [c.tst[s]], [c.tst[s]])
            TT("pool", c.st[s][:, 0:1], c.st[s][:, 1:2], mhalf[:, 0:1], ALU.pow, [c.tst[s]], [c.tst[s]])

        def transpose_tile(xn_ap, txn, pp_i, dst3, tdst, evac_eng):
            psT = PP[pp_i][:, :].bitcast(BF16)
            tb = [TB[2 * pp_i], TB[2 * pp_i + 1]]
            for cc in range(16):
                P.op("pe", lambda e, cc=cc: e.transpose(out=psT[:, cc * 128:(cc + 1) * 128],
                                                         in_=xn_ap[:, cc * 128:(cc + 1) * 128], identity=ident),
                     r=[txn], w=tb)
            src3 = psT.rearrange("p (c t) -> p c t", c=16)
            if evac_eng == "act":
                ACT(dst3, src3, AF.Copy, tb, [tdst])
            else:
                P.op("dve", lambda e: e.tensor_copy(out=dst3, in_=src3), r=tb, w=[tdst])

        def front1(c, src_dram):
            s = c.n % c.nslots
            c.n += 1
            DMA("sp", c.xt[s], src_dram, [], [c.txt[s]], c.key + "x%d" % s)
            norm_stats(c, s, c.xt[s], c.txt[s])
            return s

        def front2(c, s, dst3, tdst, pp_i, evac_eng="act"):
            xs = c.nx % 2
            c.nx += 1
            STT(c.xn[xs], c.xt[s], c.st[s][:, 0:1], c.gbc, ALU.mult, ALU.mult, [c.txt[s], c.tst[s], c.tg], [c.txn[xs]])
            transpose_tile(c.xn[xs], c.txn[xs], pp_i, dst3, tdst, evac_eng)

        class FrontPipe:
            def __init__(self, ctx, tiles):
                self.ctx = ctx
                self.tiles = tiles
                self.slot = {}

            def f1(self, k):
                if k < len(self.tiles) and k not in self.slot:
                    self.slot[k] = front1(self.ctx, self.tiles[k][0])

            def emit(self, k):
                self.f1(k)
                front2(self.ctx, self.slot[k], *self.tiles[k][1:])
                self.f1(k + 1)

        zt = AR.alloc(4096, BF16)
        tz = Tok()
        P.op("pool", lambda e: e.memset(zt, 0.0), w=[tz])
        XSz = XS.rearrange("(p r) d -> p (r d)", p=128)
        for zi in range(NE * CAP * D // 128 // 4096):
            DMA("pool", XSz[:, zi * 4096:(zi + 1) * 4096], zt, [tz], [], "Z%d" % (zi % 2))
        WA = AR.alloc(16 * 2816, BF16).rearrange("p (c n) -> p c n", c=16)
        tWA = Tok()
        wA3 = wA.rearrange("(c p) n -> p c n", p=128)
        for c0 in range(0, 2816, 704):
            DMA("pool", WA[:, :, c0:c0 + 704], wA3[:, :, c0:c0 + 704], [], [tWA], "WA")
        nA = make_norm_ctx(nmix, "A", nslots=3, sep_junk=True)
        hTg = [AR.alloc(16 * 512, BF16).rearrange("p (c t) -> p c t", c=16) for _ in range(2)]
        thT = [[Tok() for _ in range(4)] for _ in range(2)]
        KTst = [AR.alloc(4 * 512, BF16).rearrange("p (h t) -> p h t", h=4) for _ in range(2)]
        tKT = [[Tok() for _ in range(4)] for _ in range(2)]
        Vst = [AR.alloc(4 * 512, BF16).rearrange("p (t n) -> p t n", t=4) for _ in range(2)]
        tV = [[Tok() for _ in range(4)] for _ in range(2)]
        tKTs = [Tok() for _ in range(NG)]
        tVs = [Tok() for _ in range(NG)]
        pipeA = FrontPipe(nA, [(xb[k], hTg[(k // 4) % 2][:, :, (k % 4) * 128:(k % 4 + 1) * 128], thT[(k // 4) % 2][k % 4], k % 2)
                               for k in range(128)])
        for t in range(4):
            pipeA.emit(t)
        for g in range(NG):
            s = g % 2
            for t in range(4):
                if g + 1 < NG:
                    pipeA.emit(4 * (g + 1) + t)
                bk = 4 + (t % 2)
                for cc in range(16):
                    MM(bank(bk), WA[:, cc, t * 128:(t + 1) * 128], hTg[s][:, cc, :], cc == 0, cc == 15,
                       [tWA] + thT[s], [TB[bk]])
                ACT(KTst[s][:, t, :], bank(bk), AF.Copy, [TB[bk]], [tKT[s][t]])
                bv = 6 + (t % 2)
                for cc in range(16):
                    MM(bank(bv), hTg[s][:, cc, t * 128:(t + 1) * 128], WA[:, cc, 512:1024], cc == 0, cc == 15,
                       [tWA, thT[s][t]], [TB[bv]])
                ACT(Vst[s][:, t, :], bank(bv), AF.Copy, [TB[bv]], [tV[s][t]])
            DMA("pool", KTs[g], KTst[s].rearrange("p h t -> p (h t)"), tKT[s], [tKTs[g]], "KTst%d" % s)
            DMA("pool", Vs[g], Vst[s].rearrange("p t n -> p (t n)"), tV[s], [tVs[g]], "Vst%d" % s)

        QsbSt = [AR.alloc(512, BF16) for _ in range(2)]
        tQsbSt = [Tok() for _ in range(2)]
        QswSt = [AR.alloc(1024, BF16) for _ in range(2)]
        tQswSt = [Tok() for _ in range(2)]
        KswSt = [AR.alloc(256, BF16) for _ in range(2)]
        tKswSt = [Tok() for _ in range(2)]
        VswSt = [AR.alloc(256, BF16) for _ in range(2)]
        tVswSt = [Tok() for _ in range(2)]
        tHT = [Tok() for _ in range(NJ)]
        tQTsb = [Tok() for _ in range(NJ)]
        tQTsw = [Tok() for _ in range(NJ)]
        tKTsw = [Tok() for _ in range(NJ)]
        tVsw = [Tok() for _ in range(NJ)]

        tilesA2 = []
        for j in range(NJ):
            tilesA2.append((xp[j], hTg[j % 2][:, :, 0:128], thT[j % 2][0], 0))
            tilesA2.append((xo[j], hTg[j % 2][:, :, 128:256], thT[j % 2][1], 1))
        pipeA2 = FrontPipe(nA, tilesA2)

        def frontA2(j):
            pipeA2.emit(2 * j)
            pipeA2.emit(2 * j + 1)

        frontA2(0)
        for j in range(NJ):
            s = j % 2
            if j + 1 < NJ:
                frontA2(j + 1)
            DMA("pool", HT[j].rearrange("p (c t) -> p c t", c=16), hTg[s][:, :, 128:256], [thT[s][1]], [tHT[j]], "hTo%d" % s)
            for h in range(4):
                for cc in range(16):
                    MM(bank(4)[:, h * 128:(h + 1) * 128], WA[:, cc, 1024 + h * 128:1024 + (h + 1) * 128],
                       hTg[s][:, cc, 128:256], cc == 0, cc == 15, [tWA, thT[s][1]], [TB[4]])
            ACT(QsbSt[s], bank(4), AF.Copy, [TB[4]], [tQsbSt[s]])
            DMA("pool", QTsb[j], QsbSt[s], [tQsbSt[s]], [tQTsb[j]], "Qsb%d" % s)
            for gq in range(8):
                for cc in range(16):
                    MM(PP[3][:, gq * 128:(gq + 1) * 128], WA[:, cc, 1536 + gq * 128:1536 + (gq + 1) * 128],
                       hTg[s][:, cc, 128:256], cc == 0, cc == 15, [tWA, thT[s][1]], [TB[6], TB[7]])
            ACT(QswSt[s], PP[3][:, :], AF.Copy, [TB[6], TB[7]], [tQswSt[s]])
            DMA("pool", QTsw[j], QswSt[s], [tQswSt[s]], [tQTsw[j]], "Qsw%d" % s)
            for cc in range(16):
                MM(bank(5)[:, 0:256], WA[:, cc, 2560:2688], hTg[s][:, cc, 0:256], cc == 0, cc == 15,
                   [tWA, thT[s][0], thT[s][1]], [TB[5]])
            for wch in range(2):
                for cc in range(16):
                    MM(bank(5)[:, 256 + wch * 128:256 + (wch + 1) * 128], hTg[s][:, cc, wch * 128:(wch + 1) * 128],
                       WA[:, cc, 2688:2816], cc == 0, cc == 15, [tWA, thT[s][wch]], [TB[5]])
            ACT(KswSt[s], bank(5)[:, 0:256], AF.Copy, [TB[5]], [tKswSt[s]])
            ACT(VswSt[s], bank(5)[:, 256:512], AF.Copy, [TB[5]], [tVswSt[s]])
            DMA("pool", KTsw[j], KswSt[s], [tKswSt[s]], [tKTsw[j]], "Ksw%d" % s)
            DMA("pool", Vsw[j], VswSt[s], [tVswSt[s]], [tVsw[j]], "Vsw%d" % s)

        P.barrier()
        AR.release(base_mark)
        if stop_after == "A":
            return finish(nc, P, st, out, None)

        Msb = AR.alloc(512, F32).rearrange("p (t q) -> p t q", t=4)
        tM = Tok()
        DMA("sp", Msb, sbmask, [], [tM], "Cc")
        biasT = [AR.alloc(16 * 256, F32).rearrange("p (h k) -> p h k", h=16) for _ in range(2)]
        tBias = Tok()
        for v in range(2):
            DMA("sp", biasT[v], swbias[v], [], [tBias], "Cc")
        sinkb = AR.alloc(16, F32)
        tSink = Tok()
        DMA("sp", sinkb, sinks.to_broadcast([128, 16]), [], [tSink], "Cc")
        Qsb = [AR.alloc(512, BF16) for _ in range(2)]
        tQsb = [Tok() for _ in range(2)]
        NKV = 7
        KTg = [AR.alloc(2048, BF16).rearrange("p (h t) -> p h t", h=4) for _ in range(NKV)]
        tKTg = [Tok() for _ in range(NKV)]
        Vg = [AR.alloc(2048, BF16).rearrange("p (t n) -> p t n", t=4) for _ in range(NKV)]
        tVg = [Tok() for _ in range(NKV)]
        eb = [AR.alloc(1024, F32) for _ in range(4)]
        teb = [Tok() for _ in range(4)]
        spb = [AR.alloc(1024, BF16) for _ in range(4)]
        tspb = [Tok() for _ in range(4)]
        accb = [AR.alloc(512, BF16) for _ in range(4)]
        taccb = [Tok() for _ in range(4)]
        sumb = [AR.alloc(512, BF16) for _ in range(4)]
        tsumb = [Tok() for _ in range(4)]
        E2b = [AR.alloc(1024, F32) for _ in range(2)]
        tE2b = [Tok() for _ in range(2)]
        aTb = [AR.alloc(1024, BF16) for _ in range(4)]
        taTb = [Tok() for _ in range(4)]
        OsbSt = [AR.alloc(512, BF16) for _ in range(2)]
        tOsbSt = [Tok() for _ in range(2)]
        tOTsb = [Tok() for _ in range(NJ)]
        tOTsw = [Tok() for _ in range(NJ)]
        Qsw = [AR.alloc(1024, BF16).rearrange("p (g q) -> p g q", g=8) for _ in range(2)]
        tQsw = [Tok() for _ in range(2)]
        Kbd = [AR.alloc(512, BF16) for _ in range(2)]
        tKbd = [Tok() for _ in range(2)]
        Vp = [[AR.alloc(256, BF16).rearrange("p (w n) -> p w n", w=2) for _ in range(2)] for _ in range(2)]
        tVp = [Tok() for _ in range(2)]
        scb = [AR.alloc(512, F32) for _ in range(4)]
        tscb = [Tok() for _ in range(4)]
        pb = [AR.alloc(512, F32) for _ in range(4)]
        tpb = [Tok() for _ in range(4)]
        pnb = [AR.alloc(512, BF16) for _ in range(4)]
        tpnb = [Tok() for _ in range(4)]
        pTb = [AR.alloc(512, BF16) for _ in range(4)]
        tpTb = [Tok() for _ in range(4)]
        smal = [AR.alloc(16, F32) for _ in range(4)]
        tsmal = [Tok() for _ in range(4)]
        OswSt = [AR.alloc(1024, BF16) for _ in range(2)]
        tOswSt = [Tok() for _ in range(2)]
        for s in range(2):
            P.op("pool", lambda e, s=s: e.memset(Kbd[s], 0.0), w=[tKbd[s]])
            for kv in range(2):
                P.op("pool", lambda e, s=s, kv=kv: e.memset(Vp[s][kv].rearrange("p w n -> p (w n)"), 0.0), w=[tVp[s]])

        SCALE_SB = 1.0 / np.sqrt(128.0)
        SCALE_SW = 1.0 / 8.0
        nswa = [0]

        WFtmp = [AR.alloc(5632, BF16) for _ in range(2)]
        tWFtmp = [[Tok() for _ in range(3)] for _ in range(2)]

        def precast_step(j):
            if 1 <= j <= 16:
                f = j - 1
                DMA("pool", WFb[f], WFtmp[f % 2], tWFtmp[f % 2], [], "CWFo%d" % (f % 2))
            if j < 16:
                f = j
                for pi, (c0, c1) in enumerate(((0, 2048), (2048, 4096), (4096, 5632))):
                    DMA("pool", WFtmp[f % 2][:, c0:c1], wF[f][:, c0:c1], [], [tWFtmp[f % 2][pi]], "CWFi%d_%d" % (f % 2, pi))

        NSW = 4

        def swa_micro(j, g, G):
            s = j % 2
            bT = biasT[0 if j == 0 else 1]
            k = G % NSW
            half, gi = g // 4, g % 4
            sm = smal[k]
            sc3 = scb[k].rearrange("p (h k) -> p h k", h=2)
            tk = tsmal[k]

            def loads():
                precast_step(j)
                DMA("sp", Qsw[s].rearrange("p g q -> p (g q)"), QTsw[j], [tQTsw[j]], [tQsw[s]], "CQsw%d" % s)
                DMA("sp", Kbd[s][0:64, 0:256], KTsw[j][0:64, :], [tKTsw[j]], [tKbd[s]], "CKbd%d" % s)
                DMA("sp", Kbd[s][64:128, 256:512], KTsw[j][64:128, :], [tKTsw[j]], [tKbd[s]], "CKbd%d" % s)
                vsrc = Vsw[j].rearrange("p (w n) -> p w n", w=2)
                DMA("sp", Vp[s][0][:, :, 0:64], vsrc[:, :, 0:64], [tVsw[j]], [tVp[s]], "CVp%d" % s)
                DMA("sp", Vp[s][1][:, :, 64:128], vsrc[:, :, 64:128], [tVsw[j]], [tVp[s]], "CVp%d" % s)

            def m0():
                if g == 0:
                    loads()
                MM(bank(5), Qsw[s][:, g, :], Kbd[s], True, True, [tQsw[s], tKbd[s]], [TB[5]])

            def m1():
                STT(sc3, bank(5).rearrange("p (h k) -> p h k", h=2), SCALE_SW, bT[:, 2 * g:2 * g + 2, :],
                    ALU.mult, ALU.add, [TB[5], tBias], [tscb[k]])

            def m2():
                P.op("dve", lambda e: e.tensor_reduce(out=sm[:, 0:2], in_=sc3, axis=AX.X, op=ALU.max), r=[tscb[k]], w=[tk])

            def m3():
                TT("dve", sm[:, 0:2], sm[:, 0:2], sinkb[:, 2 * g:2 * g + 2], ALU.max, [tk, tSink], [tk])

            def m4():
                TS("dve", sm[:, 2:4], sm[:, 0:2], -1.0, None, ALU.mult, ALU.bypass, [tk], [tk])

            def m5():
                TT("dve", sm[:, 6:8], sinkb[:, 2 * g:2 * g + 2], sm[:, 2:4], ALU.add, [tk, tSink], [tk])

            def m6():
                for hh in range(2):
                    ACT(pb[k][:, hh * 256:(hh + 1) * 256], scb[k][:, hh * 256:(hh + 1) * 256], AF.Exp,
                        [tscb[k], tk], [tpb[k], tk], bias=sm[:, 2 + hh:3 + hh], accum_out=sm[:, 4 + hh:5 + hh])
                ACT(sm[:, 6:8], sm[:, 6:8], AF.Exp, [tk], [tk])

            def m7():
                TT("dve", sm[:, 8:10], sm[:, 6:8], sm[:, 4:6], ALU.add, [tk], [tk])

            def m8():
                P.op("dve", lambda e: e.reciprocal(out=sm[:, 10:12], in_=sm[:, 8:10]), r=[tk], w=[tk])

            def m9():
                for hh in range(2):
                    TS("dve", pnb[k][:, hh * 256:(hh + 1) * 256], pb[k][:, hh * 256:(hh + 1) * 256],
                       sm[:, 10 + hh:11 + hh], None, ALU.mult, ALU.bypass, [tpb[k], tk], [tpnb[k]])

            def m10():
                pT = bank(6).bitcast(BF16)[:, 0:512]
                for q4 in range(4):
                    P.op("pe", lambda e, q4=q4: e.transpose(out=pT[:, q4 * 128:(q4 + 1) * 128],
                                                             in_=pnb[k][:, q4 * 128:(q4 + 1) * 128], identity=ident),
                         r=[tpnb[k]], w=[TB[6]])

            def m11():
                pT = bank(6).bitcast(BF16)[:, 0:512]
                P.op("dve", lambda e: e.tensor_copy(out=pTb[k], in_=pT), r=[TB[6]], w=[tpTb[k]])

            def m12():
                oc = bank(7)[:, gi * 128:(gi + 1) * 128]
                for q4 in range(4):
                    kv, kt = q4 // 2, q4 % 2
                    MM(oc, Vp[s][kv][:, kt, :], pTb[k][:, q4 * 128:(q4 + 1) * 128], q4 == 0, q4 == 3,
                       [tVp[s], tpTb[k]], [TB[7]])

            def m13():
                if gi == 3:
                    ACT(OswSt[s][:, half * 512:(half + 1) * 512], bank(7), AF.Copy, [TB[7]], [tOswSt[s]])
                    if half == 1:
                        DMA("pool", OTsw[j], OswSt[s], [tOswSt[s]], [tOTsw[j]], "COsw%d" % s)

            return [m0, m1, m2, m3, m4, m5, m6, m7, m8, m9, m10, m11, m12, m13]

        swa_all = [swa_micro(j, g, j * 8 + g) for j in range(NJ) for g in range(8)]
        NMS = 14
        SW_SP = 4
        swa_T = [0]
        SWA_TICKS = SW_SP * (len(swa_all) - 1) + NMS

        def swa_tick():
            T = swa_T[0]
            swa_T[0] += 1
            if T >= SWA_TICKS:
                return
            G0 = max(0, (T - NMS + SW_SP) // SW_SP)
            for G in range(G0, min(len(swa_all), T // SW_SP + 1)):
                m = T - SW_SP * G
                if 0 <= m < NMS:
                    swa_all[G][m]()

        its = []
        for j in range(NJ):
            for gq in range(j, -1, -1):
                for tp in (1, 0):
                    its.append((j, gq, tp))
        NIT = len(its)
        slot_of = {}
        nload = [0]
        Sbuf = {}
        NB4 = 4
        psZ = PP[0]
        psC = PP[1]
        tZ = [TB[0], TB[1]]
        tCb = [TB[2], TB[3]]

        def stageZ1(n):
            j, gq, tp = its[n]
            first = (gq == j and tp == 1)
            js = j % 2
            if first:
                if j == 0:
                    DMA("sp", Qsb[0], QTsb[0], [tQTsb[0]], [tQsb[0]], "CQsb0")
                if j + 1 < NJ:
                    jn = (j + 1) % 2
                    DMA("sp", Qsb[jn], QTsb[j + 1], [tQTsb[j + 1]], [tQsb[jn]], "CQsb%d" % jn)
            if tp == 1:
                sl = nload[0] % NKV
                nload[0] += 1
                slot_of[(j, gq)] = sl
                DMA("sp", KTg[sl].rearrange("p h t -> p (h t)"), KTs[gq], [tKTs[gq]], [tKTg[sl]], "CKT%d" % sl)
                DMA("sp", Vg[sl].rearrange("p t n -> p (t n)"), Vs[gq], [tVs[gq]], [tVg[sl]], "CV%d" % sl)
            sl = slot_of[(j, gq)]
            es = n % NB4
            for u in range(2):
                t = 2 * tp + 1 - u
                for h in range(4):
                    MM(psZ[:, u * 512 + h * 128:u * 512 + (h + 1) * 128], KTg[sl][:, h, t * 128:(t + 1) * 128],
                       Qsb[js][:, h * 128:(h + 1) * 128], True, True, [tKTg[sl], tQsb[js]], tZ)
            ACT(eb[es], psZ[:, :], AF.Exp, tZ, [teb[es]], scale=float(SCALE_SB))
            if gq == j:
                for u in range(2):
                    t = 2 * tp + 1 - u
                    e3 = eb[es][:, u * 512:(u + 1) * 512].rearrange("p (h q) -> p h q", h=4)
                    TT("dve", e3, e3, Msb[:, t, :].unsqueeze(1).to_broadcast([128, 4, 128]), ALU.mult, [teb[es], tM], [teb[es]])

        def stageZ2(n):
            j, gq, tp = its[n]
            first = (gq == j and tp == 1)
            last = (gq == 0 and tp == 0)
            es = n % NB4
            ACT(spb[es], eb[es], AF.Ln, [teb[es]], [tspb[es]], bias=1.0)
            if not last:
                a = n % NB4
                sp_hi = spb[es][:, 0:512]
                sp_lo = spb[es][:, 512:1024]
                if first:
                    TT("dve", accb[a], sp_hi, sp_lo, ALU.add, [tspb[es]], [taccb[a]])
                else:
                    pa, pt = Sbuf[n - 1]
                    TT("dve", sumb[a], sp_hi, sp_lo, ALU.add, [tspb[es]], [tsumb[a]])
                    TT("dve", accb[a], pa, sumb[a], ALU.add, [pt, tsumb[a]], [taccb[a]])
                Sbuf[n] = (accb[a], taccb[a])

        def stageC1(n):
            j, gq, tp = its[n]
            first = (gq == j and tp == 1)
            es = n % NB4
            sp_hi = spb[es][:, 0:512]
            sp_lo = spb[es][:, 512:1024]
            hi = psC[:, 0:512]
            lo = psC[:, 512:1024]
            MM(hi, uincl, sp_hi, True, first, [tspb[es]], tCb)
            if not first:
                pa, pt = Sbuf[n - 1]
                MM(hi, ones, pa, False, True, [pt], tCb)
            MM(lo, uincl, sp_lo, True, False, [tspb[es]], tCb)
            MM(lo, ones, sp_hi, False, first, [tspb[es]], tCb)
            if not first:
                MM(lo, ones, pa, False, True, [pt], tCb)
            k = n % 2
            ACT(E2b[k], psC[:, :], AF.Exp, tCb, [tE2b[k]], scale=-1.0)

        def stageC2(n):
            es = n % NB4
            k = n % 2
            TT("dve", aTb[es], eb[es], E2b[k], ALU.mult, [teb[es], tE2b[k]], [taTb[es]])

        def stageAV(n):
            j, gq, tp = its[n]
            first = (gq == j and tp == 1)
            last = (gq == 0 and tp == 0)
            sl = slot_of[(j, gq)]
            es = n % NB4
            js = j % 2
            for u in range(2):
                t = 2 * tp + 1 - u
                for h in range(4):
                    MM(bank(4)[:, h * 128:(h + 1) * 128], Vg[sl][:, t, h * 128:(h + 1) * 128],
                       aTb[es][:, u * 512 + h * 128:u * 512 + (h + 1) * 128],
                       first and u == 0 and h == 0, last and u == 1, [tVg[sl], taTb[es]], [TB[4]])
            if last:
                P.op("dve", lambda e, js=js: e.tensor_copy(out=OsbSt[js], in_=bank(4)), r=[TB[4]], w=[tOsbSt[js]])
                DMA("pool", OTsb[j], OsbSt[js], [tOsbSt[js]], [tOTsb[j]], "COsb%d" % js)

        SKA, SKB = 2, 4
        for n in range(NIT + SKB):
            if n < NIT:
                stageZ1(n)
            if SKA <= n < NIT + SKA:
                stageC1(n - SKA)
            if n < NIT:
                stageZ2(n)
            if SKA <= n < NIT + SKA:
                stageC2(n - SKA)
            if n >= SKB:
                stageAV(n - SKB)
            swa_tick()
        while swa_T[0] < SWA_TICKS:
            swa_tick()

        P.barrier()
        AR.release(base_mark)
        if stop_after == "C":
            return finish(nc, P, st, out, None)

        WO = AR.alloc(16 * D, BF16).rearrange("p (c n) -> p c n", c=16)
        WRs = AR.alloc(16 * 36, BF16).rearrange("p (c n) -> p c n", c=16)
        tWD = Tok()
        wO3 = wOut.rearrange("(c p) n -> p c n", p=128)
        for c0 in range(0, 16, 4):
            DMA("pool", WO[:, c0:c0 + 4, :], wO3[:, c0:c0 + 4, :], [], [tWD], "DW")
        DMA("pool", WRs, wR.rearrange("(c p) n -> p c n", p=128), [], [tWD], "DW")
        bRb = AR.alloc(36, F32)
        erowb = AR.alloc(NE, F32)
        DMA("sp", bRb, bR.to_broadcast([128, 36]), [], [tWD], "Dc")
        DMA("sp", erowb, erow.to_broadcast([128, NE]), [], [tWD], "Dc")
        nD = make_norm_ctx(nffn, "D", sep_junk=True)
        OTsbg = [AR.alloc(4 * 512, BF16).rearrange("p (c t) -> p c t", c=4) for _ in range(2)]
        OTswg = [AR.alloc(8 * 512, BF16).rearrange("p (c t) -> p c t", c=8) for _ in range(2)]
        hTog1 = AR.alloc(16 * 512, BF16).rearrange("p (c t) -> p c t", c=16)
        hTog = [hTog1, hTog1]
        tIn = [[Tok(), Tok()] for _ in range(2)]
        tInH = Tok()
        for s_ in range(2):
            tIn[s_].append(tInH)
        WFs = [AR.alloc(5632, BF16) for _ in range(2)]
        tWGp = [[Tok() for _ in range(3)] for _ in range(2)]
        mT = AR.alloc(16 * 512, BF16).rearrange("p (c t) -> p c t", c=16)
        tmT = [Tok() for _ in range(16)]
        sg = [AR.alloc(512, BF16) for _ in range(2)]
        tsg = [Tok() for _ in range(2)]
        tt1 = AR.alloc(512, F32)
        ttt1 = Tok()
        tt2 = AR.alloc(512, F32)
        ttt2 = Tok()
        h2T = [AR.alloc(16 * 128, BF16).rearrange("p (c t) -> p c t", c=16) for _ in range(2)]
        th2T = [Tok() for _ in range(2)]
        rt = [AR.alloc(256, F32) for _ in range(2)]
        trt = [Tok() for _ in range(2)]
        selb = [AR.alloc(NE, BF16) for _ in range(2)]
        tselb = [Tok() for _ in range(2)]
        selacc = [AR.alloc(NE, BF16) for _ in range(2)]
        tselacc = [Tok() for _ in range(2)]
        tX1 = [Tok() for _ in range(NJ)]
        tXS = Tok()
        tIdx = Tok()
        nwg = [0]
        for tg in range(NJ // 4):
            s = tg % 2
            for jj in range(4):
                j = 4 * tg + jj
                DMA("sp", OTsbg[s][:, :, jj * 128:(jj + 1) * 128], OTsb[j].rearrange("p (h q) -> p h q", h=4), [tOTsb[j]], [tIn[s][0]], "DI0%d" % s)
                DMA("sp", OTswg[s][:, :, jj * 128:(jj + 1) * 128], OTsw[j].rearrange("p (g q) -> p g q", g=8), [tOTsw[j]], [tIn[s][1]], "DI1%d" % s)
                DMA("sp", hTog[s][:, :, jj * 128:(jj + 1) * 128], HT[j].rearrange("p (c t) -> p c t", c=16), [tHT[j]], [tIn[s][2]], "DI2")
            for f in range(16):
                ws = nwg[0] % 2
                bo = 4 * (f % 2)
                nwg[0] += 1
                for (c0, c1) in ((0, 2048), (2048, 4096), (4096, 5632)):
                    DMA("sp", WFs[ws][:, c0:c1], WFb[f][:, c0:c1], [], [tWGp[ws][c0 // 2048]], "DWG%d_%d" % (ws, c0))
                WGv = WFs[ws][:, 0:4096].rearrange("p (c n) -> p c n", c=16)
                WUv = WFs[ws][:, 4096:5632].rearrange("p (c n) -> p c n", c=12)
                for kc in range(4):
                    MM(bank(bo + 0), WUv[:, kc, :], OTsbg[s][:, kc, :], kc == 0, kc == 3, [tWGp[ws][2], tIn[s][0]], [TB[bo + 0]])
                for kc in range(8):
                    MM(bank(bo + 1), WUv[:, 4 + kc, :], OTswg[s][:, kc, :], kc == 0, kc == 7, [tWGp[ws][2], tIn[s][1]], [TB[bo + 1]])
                for gsel in range(2):
                    for cc in range(16):
                        MM(bank(bo + 2 + gsel), WGv[:, cc, gsel * 128:(gsel + 1) * 128], hTog[s][:, cc, :], cc == 0, cc == 15,
                           [tWGp[ws][cc // 8], tIn[s][2]], [TB[bo + 2 + gsel]])
                ACT(sg[0], bank(bo + 2), AF.Sigmoid, [TB[bo + 2]], [tsg[0]])
                ACT(sg[1], bank(bo + 3), AF.Sigmoid, [TB[bo + 3]], [tsg[1]])
                TT("dve", tt1, sg[0], bank(bo + 0), ALU.mult, [tsg[0], TB[bo + 0]], [ttt1])
                TT("dve", tt2, sg[1], bank(bo + 1), ALU.mult, [tsg[1], TB[bo + 1]], [ttt2])
                TT("pool", mT[:, f, :], tt1, tt2, ALU.add, [ttt1, ttt2], [tmT[f]])
            def part1a(jj, tg=tg):
                j = 4 * tg + jj
                xs_ = nD.n % 2
                nD.n += 1
                xtile = nD.xt[xs_]
                txt = nD.txt[xs_]
                DMA("sp", xtile, xo[j], [], [txt], "Dx%d" % xs_)
                for nn in range(4):
                    bx = 4 + nn % 2
                    for f in range(16):
                        MM(bank(bx), mT[:, f, jj * 128:(jj + 1) * 128], WO[:, f, nn * 512:(nn + 1) * 512], f == 0, f == 15,
                           [tmT[f], tWD], [TB[bx]])
                    TT("dve", xtile[:, nn * 512:(nn + 1) * 512], bank(bx), xtile[:, nn * 512:(nn + 1) * 512], ALU.add,
                       [TB[bx], txt], [txt])
                DMA("pool", X1[j], xtile, [txt], [tX1[j]], "Dx%d" % xs_)
                norm_stats(nD, xs_, xtile, txt)
                return dict(j=j, xs_=xs_, xtile=xtile, txt=txt)

            def part1b1(sta):
                j, xs_, xtile, txt = sta['j'], sta['xs_'], sta['xtile'], sta['txt']
                h2 = nD.xn[xs_]
                th2 = nD.txn[xs_]
                STT(h2, xtile, nD.st[xs_][:, 0:1], nD.gbc, ALU.mult, ALU.mult, [txt, nD.tst[xs_], nD.tg], [th2])

            def part1b(sta):
                j, xs_, xtile, txt = sta['j'], sta['xs_'], sta['xtile'], sta['txt']
                h2 = nD.xn[xs_]
                th2 = nD.txn[xs_]
                hs = j % 2
                transpose_tile(h2, th2, 3, h2T[hs], th2T[hs], "act")
                br_ = 4 + j % 2
                for cc in range(16):
                    MM(bank(br_)[:, 0:36], h2T[hs][:, cc, :], WRs[:, cc, :], cc == 0, cc == 15, [th2T[hs], tWD], [TB[br_]])
                R = rt[hs]
                tR = trt[hs]
                lg = R[:, 0:36]
                TT("dve", lg, bank(br_)[:, 0:36], bRb, ALU.add, [TB[br_], tWD], [tR])
                return dict(j=j, hs=hs, R=R, tR=tR, lg=lg, h2=h2, th2=th2, xs_=xs_)

            def part2(stt):
                j, hs, R, tR, lg, h2, th2, xs_ = (stt[k_] for k_ in ('j', 'hs', 'R', 'tR', 'lg', 'h2', 'th2', 'xs_'))
                rb = j % 2
                gmax = R[:, 36:37]
                P.op("dve", lambda e, lg=lg, gmax=gmax: e.tensor_reduce(out=gmax, in_=lg[:, 0:4], axis=AX.X, op=ALU.max), r=[tR], w=[tR])
                gm = R[:, 40:44]
                TS("dve", gm, lg[:, 0:4], gmax, None, ALU.is_ge, ALU.bypass, [tR], [tR])
                gd = R[:, 44:48]
                TS("dve", gd, lg[:, 0:4], gmax, None, ALU.subtract, ALU.bypass, [tR], [tR])
                gsum = R[:, 37:38]
                ACT(gd, gd, AF.Exp, [tR], [tR], accum_out=gsum)
                pgrp = R[:, 38:39]
                P.op("dve", lambda e, pgrp=pgrp, gsum=gsum: e.reciprocal(out=pgrp, in_=gsum), r=[tR], w=[tR])
                pen = R[:, 48:52]
                TS("dve", pen, gm, BIG, -BIG, ALU.mult, ALU.add, [tR], [tR])
                ml = R[:, 64:96]
                TT("dve", ml.rearrange("p (g k) -> p g k", g=4), lg[:, 4:36].rearrange("p (g k) -> p g k", g=4),
                   pen.unsqueeze(2).to_broadcast([128, 4, 8]), ALU.add, [tR], [tR])
                m1 = R[:, 52:53]
                P.op("dve", lambda e, ml=ml, m1=m1: e.tensor_reduce(out=m1, in_=ml, axis=AX.X, op=ALU.max), r=[tR], w=[tR])
                is1 = R[:, 96:128]
                TS("dve", is1, ml, m1, None, ALU.is_ge, ALU.bypass, [tR], [tR])
                ml2 = R[:, 128:160]
                STT(ml2, is1, -BIG, ml, ALU.mult, ALU.add, [tR], [tR])
                m2 = R[:, 53:54]
                P.op("dve", lambda e, ml2=ml2, m2=m2: e.tensor_reduce(out=m2, in_=ml2, axis=AX.X, op=ALU.max), r=[tR], w=[tR])
                is2 = R[:, 160:192]
                TS("dve", is2, ml2, m2, None, ALU.is_ge, ALU.bypass, [tR], [tR])
                d21 = R[:, 54:55]
                TT("dve", d21, m2, m1, ALU.subtract, [tR], [tR])
                e21 = R[:, 55:56]
                ACT(e21, d21, AF.Exp, [tR], [tR])
                den = R[:, 56:57]
                TS("dve", den, e21, 1.0, None, ALU.add, ALU.bypass, [tR], [tR])
                w1 = R[:, 57:58]
                P.op("dve", lambda e, w1=w1, den=den: e.reciprocal(out=w1, in_=den), r=[tR], w=[tR])
                TT("dve", gt1[:, j:j + 1], w1, pgrp, ALU.mult, [tR], [tIdx])
                TT("dve", gt2[:, j:j + 1], gt1[:, j:j + 1], e21, ALU.mult, [tR, tIdx], [tIdx])
                TT("dve", selb[hs], is1, is2, ALU.add, [tR], [tselb[hs]])
                MM(bank(rb)[:, 0:32], ustrict, selb[hs], True, j == 0, [tselb[hs]], [TB[rb]])
                if j > 0:
                    MM(bank(rb)[:, 0:32], ones, selacc[(j - 1) % 2], False, True, [tselacc[(j - 1) % 2]], [TB[rb]])
                if j == 0:
                    P.op("pool", lambda e, hs=hs: e.tensor_copy(out=selacc[0], in_=selb[hs]), r=[tselb[hs]], w=[tselacc[0]])
                else:
                    TT("pool", selacc[j % 2], selacc[(j - 1) % 2], selb[hs], ALU.add, [tselacc[(j - 1) % 2], tselb[hs]], [tselacc[j % 2]])
                dest = R[:, 192:224]
                TT("dve", dest, bank(rb)[:, 0:32], erowb, ALU.add, [TB[rb], tWD], [tR])
                i1f = R[:, 58:59]
                i2f = R[:, 59:60]
                junk32 = R[:, 224:256]
                STT(junk32, is1, 1.0, dest, ALU.mult, ALU.mult, [tR], [tR], accum_out=i1f)
                STT(junk32, is2, 1.0, dest, ALU.mult, ALU.mult, [tR], [tR], accum_out=i2f)
                P.op("dve", lambda e, j=j, i1f=i1f: e.tensor_copy(out=idx1[:, j:j + 1], in_=i1f), r=[tR], w=[tIdx])
                P.op("dve", lambda e, j=j, i2f=i2f: e.tensor_copy(out=idx2[:, j:j + 1], in_=i2f), r=[tR], w=[tIdx])
                for ix in (idx1, idx2):
                    P.op("pool", lambda e, ix=ix, j=j, h2=h2: e.indirect_dma_start(
                        out=XS, out_offset=bass.IndirectOffsetOnAxis(ap=ix[:, j:j + 1], axis=0),
                        in_=h2, in_offset=None, bounds_check=bc_reg(e), oob_is_err=False),
                        r=[th2, tIdx], w=[], key="Dsc%d" % xs_)


            sa_ = [part1a(0)]
            sb_ = []
            for jj in range(4):
                part1b1(sa_[jj])
                if jj + 1 < 4:
                    sa_.append(part1a(jj + 1))
                sb_.append(part1b(sa_[jj]))
                if jj >= 1:
                    part2(sb_[jj - 1])
            part2(sb_[3])

        if dbg and "RT" in dbg:
            dI1 = nc.dram_tensor("dI1", [128, NJ], I32, kind="ExternalOutput").ap()
            dI2 = nc.dram_tensor("dI2", [128, NJ], I32, kind="ExternalOutput").ap()
            dG1 = nc.dram_tensor("dG1", [128, NJ], F32, kind="ExternalOutput").ap()
            dG2 = nc.dram_tensor("dG2", [128, NJ], F32, kind="ExternalOutput").ap()
            for dd, ss in ((dI1, idx1), (dI2, idx2), (dG1, gt1), (dG2, gt2)):
                DMA("sp", dd, ss, [tIdx], [], "Dc")
        print('arena D high', AR.off, AR.n)
        P.barrier()
        AR.release(base_mark)
        if stop_after == "D":
            return finish(nc, P, st, out, None)

        Wg_ = [AR.alloc(16 * 512, BF16).rearrange("p (c n) -> p c n", c=16) for _ in range(2)]
        Wu_ = [AR.alloc(16 * 512, BF16).rearrange("p (c n) -> p c n", c=16) for _ in range(2)]
        Wd_ = [AR.alloc(4 * D, BF16).rearrange("p (c n) -> p c n", c=4) for _ in range(2)]
        tWgp = [[Tok() for _ in range(4)] for _ in range(2)]
        tWup = [[Tok() for _ in range(4)] for _ in range(2)]
        tWdp = [[Tok() for _ in range(2)] for _ in range(2)]
        xsr = [AR.alloc(D, BF16) for _ in range(4)]
        txsr = [Tok() for _ in range(4)]
        xsT = [AR.alloc(16 * CAP, BF16).rearrange("p (c t) -> p c t", c=16) for _ in range(2)]
        txsT = [[Tok() for _ in range(NCT)] for _ in range(2)]
        sa = [AR.alloc(CAP, F32) for _ in range(2)]
        tsa = [Tok() for _ in range(2)]
        hTe = AR.alloc(4 * CAP, BF16).rearrange("p (c t) -> p c t", c=4)
        thTe = [Tok() for _ in range(4)]
        ysb = [AR.alloc(D, F32) for _ in range(2)]
        tysb = [Tok() for _ in range(2)]
        tYS = Tok()
        nxs = [0]
        nys = [0]
        def e_weights(ex):
            s = ex % 2
            wg3 = wGate[ex].rearrange("(c p) n -> p c n", p=128)
            wu3 = wUp[ex].rearrange("(c p) n -> p c n", p=128)
            wd3 = wDown[ex].rearrange("(c p) n -> p c n", p=128)
            for c0 in range(0, 16, 4):
                DMA("pool", Wg_[s][:, c0:c0 + 4, :], wg3[:, c0:c0 + 4, :], [], [tWgp[s][c0 // 4]], "EWg%d_%d" % (s, c0))
            for c0 in range(0, 16, 4):
                DMA("pool", Wu_[s][:, c0:c0 + 4, :], wu3[:, c0:c0 + 4, :], [], [tWup[s][c0 // 4]], "EWu%d_%d" % (s, c0))
            for c0 in range(0, 4, 2):
                DMA("pool", Wd_[s][:, c0:c0 + 2, :], wd3[:, c0:c0 + 2, :], [], [tWdp[s][c0 // 2]], "EWd%d_%d" % (s, c0))

        def e_rows(ex, tt):
            s = ex % 2
            k = nxs[0] % 4
            nxs[0] += 1
            DMA("sp", xsr[k], XS[ex * CAP + tt * 128:ex * CAP + (tt + 1) * 128, :], [], [txsr[k]], "Exs%d" % k)
            transpose_tile(xsr[k], txsr[k], 3, xsT[s][:, :, tt * 128:(tt + 1) * 128], txsT[s][tt], "dve" if tt % 2 else "act")

        e_weights(0)
        for tt in range(NCT):
            e_rows(0, tt)
        YB = [4, 5, 0, 1]
        for ex in range(NE):
            s = ex % 2
            if ex + 1 < NE:
                e_weights(ex + 1)
            for dc in range(4):
                for cc in range(16):
                    MM(bank(0 + dc % 2)[:, 0:CAP], Wg_[s][:, cc, dc * 128:(dc + 1) * 128], xsT[s][:, cc, :], cc == 0, cc == 15,
                       [tWgp[s][cc // 4]] + txsT[s], [TB[0 + dc % 2]])
                for cc in range(16):
                    MM(bank(2 + dc % 2)[:, 0:CAP], Wu_[s][:, cc, dc * 128:(dc + 1) * 128], xsT[s][:, cc, :], cc == 0, cc == 15,
                       [tWup[s][cc // 4]] + txsT[s], [TB[2 + dc % 2]])
                ACT(sa[dc % 2], bank(0 + dc % 2)[:, 0:CAP], AF.Silu, [TB[0 + dc % 2]], [tsa[dc % 2]])
                TT("dve", hTe[:, dc, :], sa[dc % 2], bank(2 + dc % 2)[:, 0:CAP], ALU.mult, [tsa[dc % 2], TB[2 + dc % 2]], [thTe[dc]])
            for tt in range(NCT):
                k = nys[0] % 2
                nys[0] += 1
                for nn in range(4):
                    by = YB[nn]
                    for dc in range(4):
                        MM(bank(by), hTe[:, dc, tt * 128:(tt + 1) * 128], Wd_[s][:, dc, nn * 512:(nn + 1) * 512], dc == 0, dc == 3,
                           [thTe[dc], tWdp[s][dc // 2]], [TB[by]])
                    if nn % 2 == 0:
                        ACT(ysb[k][:, nn * 512:(nn + 1) * 512], bank(by), AF.Copy, [TB[by]], [tysb[k]])
                    else:
                        P.op("dve", lambda e, k=k, nn=nn, by=by: e.tensor_copy(out=ysb[k][:, nn * 512:(nn + 1) * 512], in_=bank(by)),
                             r=[TB[by]], w=[tysb[k]])
                DMA("act", YS[ex * CAP + tt * 128:ex * CAP + (tt + 1) * 128, :], ysb[k], [tysb[k]], [], "Eys%d" % k)
                if ex + 1 < NE:
                    e_rows(ex + 1, tt)

        P.barrier()
        AR.release(base_mark)

        nF = make_norm_ctx(nfin, "F", nslots=4)
        y1 = [AR.alloc(D, F32) for _ in range(4)]
        y2 = [AR.alloc(D, F32) for _ in range(4)]
        ty = [[Tok() for _ in range(2)] for _ in range(4)]
        ob = [AR.alloc(D, F32) for _ in range(2)]
        tob = [Tok() for _ in range(2)]
        tOut = [Tok() for _ in range(NJ)]
        def f_loads(j):
            s = j % 4
            DMA("sp", nF.xt[s], X1[j], [tX1[j]], [nF.txt[s]], "Fx%d" % s)
            for yy, ix, tk, kn in ((y1, idx1, ty[s][0], "Fy1%d" % s), (y2, idx2, ty[s][1], "Fy2%d" % s)):
                P.op("pool", lambda e, yy=yy, ix=ix, j=j, s=s: e.indirect_dma_start(
                    out=yy[s], out_offset=None, in_=YS,
                    in_offset=bass.IndirectOffsetOnAxis(ap=ix[:, j:j + 1], axis=0),
                    bounds_check=bc_reg(e), oob_is_err=False),
                    r=[tIdx], w=[tk], key=kn)

        for j in range(3):
            f_loads(j)
        for j in range(NJ):
            s = j % 4
            so = j % 2
            if j + 3 < NJ:
                f_loads(j + 3)
            xt_ = nF.xt[s]
            STT(xt_, y1[s], gt1[:, j:j + 1], xt_, ALU.mult, ALU.add, [ty[s][0], nF.txt[s], tIdx], [nF.txt[s]])
            STT(xt_, y2[s], gt2[:, j:j + 1], xt_, ALU.mult, ALU.add, [ty[s][1], nF.txt[s], tIdx], [nF.txt[s]])
            norm_stats(nF, s, xt_, nF.txt[s])
            STT(ob[so], xt_, nF.st[s][:, 0:1], nF.gbc, ALU.mult, ALU.mult, [nF.txt[s], nF.tst[s], nF.tg], [tob[so]])
            DMA("sp", out[j], ob[so], [tob[so]], [tOut[j]], "Fo%d" % so)

        return finish(nc, P, st, out, tOut)


def finish(nc, P, st, out, tOut):
    if tOut is not None:
        P.op("sp", None, r=tOut)
    else:
        P.barrier()
        P.op("sp", None)
    P.emit(st)
    print('ops', len(P.ops), 'waits', P.n_waits, 'keys', len(P.key_cnt))
    return nc


def _layouts(inp):
    f32 = np.float32
    w_in = np.asarray(inp["w_in"])[0]
    qsw_src = np.arange(1024).reshape(2, 8, 64).transpose(1, 0, 2).reshape(-1)
    colsA = np.concatenate([np.arange(512, 1024), np.arange(1024, 1536), np.arange(0, 512),
                            1536 + qsw_src, np.arange(2560, 2688), np.arange(2688, 2816)])
    wA = np.ascontiguousarray(w_in[:, colsA])
    wUsw = np.asarray(inp["w_up_sw"])[0][qsw_src, :]
    wUsb = np.asarray(inp["w_up_sb"])[0]
    wF = np.empty((16, 128, 5632), f32)
    wF[:, :, 0:4096] = w_in[:, 2816:].reshape(16, 128, 2, 16, 128).transpose(3, 1, 0, 2, 4).reshape(16, 128, 4096)
    wF[:, :, 4096:4608] = wUsb.reshape(4, 128, 16, 128).transpose(2, 1, 0, 3).reshape(16, 128, 512)
    wF[:, :, 4608:5632] = wUsw.reshape(8, 128, 16, 128).transpose(2, 1, 0, 3).reshape(16, 128, 1024)
    sinks = np.asarray(inp["sinks"])[0]
    sinks_p = np.ascontiguousarray(sinks.reshape(2, 8).T.reshape(1, 16))
    wR = np.ascontiguousarray(np.concatenate([np.asarray(inp["w_router_group"])[0], np.asarray(inp["w_router_expert"])[0]], axis=1))
    bR = np.ascontiguousarray(np.concatenate([np.asarray(inp["b_router_group"])[0], np.asarray(inp["b_router_expert"])[0]])[None, :])
    common = {
        "wA": wA, "wF": wF,
        "wOut": np.ascontiguousarray(np.asarray(inp["w_out"])[0]),
        "wR": wR, "bR": bR,
        "wGate": np.ascontiguousarray(np.asarray(inp["w_gate"])[0]),
        "wUp": np.ascontiguousarray(np.asarray(inp["w_up"])[0]),
        "wDown": np.ascontiguousarray(np.asarray(inp["w_down"])[0]),
        "nmix": np.ascontiguousarray(np.asarray(inp["norm_mix"]).reshape(1, D)),
        "nffn": np.ascontiguousarray(np.asarray(inp["norm_ffn"]).reshape(1, D)),
        "nfin": np.ascontiguousarray(np.asarray(inp["norm_final"]).reshape(1, D)),
        "sinks": sinks_p,
        "erow": (np.arange(NE, dtype=f32) * CAP)[None, :],
    }
    kk = np.arange(128)[:, None]
    qq = np.arange(128)[None, :]
    tri = (kk < qq).astype(f32)
    slopes = np.exp2(-8.0 * np.arange(1, 17, dtype=np.float64) / 16.0)
    qi = np.arange(128)[:, None]
    kj = np.arange(256)[None, :]
    dist = qi + 128 - kj
    valid = (dist >= 0) & (dist < 128)
    hp = np.arange(16).reshape(2, 8).T.reshape(-1)
    bias_gen = np.where(valid[:, None, :], -slopes[hp][None, :, None] * dist[:, None, :], -1e30).astype(f32)
    bias_first = bias_gen.copy()
    bias_first[:, :, 0:128] = -1e30
    x = np.asarray(inp["x"])
    maps = []
    for c in range(8):
        b, r = c // 4, c % 4
        xbv = x[b].reshape(128, 128, D)
        x4 = x[b].reshape(32, 4, 128, D)
        xo = np.ascontiguousarray(x4[:, r])
        if r > 0:
            xp = np.ascontiguousarray(x4[:, r - 1])
        else:
            xp = np.zeros((32, 128, D), f32)
            xp[1:] = x4[:-1, 3]
        m = np.zeros((128, 4, 128), f32)
        for t in range(4):
            if t < r:
                m[:, t, :] = 1.0
            elif t == r:
                m[:, t, :] = tri
        swb = np.stack([bias_first if r == 0 else bias_gen, bias_gen]).astype(f32)
        d = dict(common)
        d.update({"xb": xbv, "xo": xo, "xp": xp, "sbmask": m, "swbias": swb})
        maps.append(d)
    return maps


_NC_CACHE = {}


def kernel(**inputs):
    maps = _layouts(inputs)
    if "nc" not in _NC_CACHE:
        _NC_CACHE["nc"] = build_nc()
    nc = _NC_CACHE["nc"]
    res = run_bass_kernel_spmd(nc, maps, core_ids=list(range(8)))
    outp = np.empty((2, S, D), np.float32)
    o5 = outp.reshape(2, 32, 4, 128, D)
    for c in range(8):
        b, r = c // 4, c % 4
        o5[b, :, r] = np.asarray(res.results[c]["out"]).reshape(32, 128, D)
    return outp
```
